# Optimizing a Trainium2 kernel written in Bass

```python
import jax, jax.numpy as jnp
from jax import lax
import numpy as np

D_MODEL = 1024
BATCH = 8
SEQ = 4096
DEPTH = 2

GRID_W = 64
CTX_LEN = 256
HEAD_DIM = 64
D_MIX = D_MODEL
CONV_CH = D_MIX // 4
NA_HEADS = (3 * D_MIX // 8) // HEAD_DIM
NA_DIM = NA_HEADS * HEAD_DIM
SW_HEADS = (D_MIX - CONV_CH - NA_DIM) // HEAD_DIM
SW_KV_HEADS = 2
SW_DIM = SW_HEADS * HEAD_DIM
SW_KV_DIM = SW_KV_HEADS * HEAD_DIM
CONV_WIDTH = 3
NA_WIN_R = 8
NA_WIN_C = 16
NA_QB = 16
NA_KB = NA_QB + NA_WIN_C
SW_RADIUS = 128
SW_BLOCK = 128
ROPE_THETA = 10000.0
D_FF = 2816
N_EXPERTS = 8
TOP_K = 2
D_FF_EXPERT = 3584
N_DENSE = (DEPTH + 1) // 2
N_MOE = DEPTH // 2
EPS = 1e-6
NEG_INF = -1e30
Q_SPLITS = [CONV_CH, 2 * CONV_CH, 3 * CONV_CH, 3 * CONV_CH + NA_DIM]
KV_OFF = 3 * CONV_CH + NA_DIM + SW_DIM
KV_SPLITS = [NA_DIM, 2 * NA_DIM, 2 * NA_DIM + SW_KV_DIM]
KV_WIDTH = 2 * NA_DIM + 2 * SW_KV_DIM
D_IN = KV_OFF + KV_WIDTH

kernel_name = "hybrid_ctxprefix_conv_natten_swa_moe"


def rms_norm(x, g):
    xf = x.astype(jnp.float32)
    y = xf * lax.rsqrt(jnp.mean(xf * xf, axis=-1, keepdims=True) + EPS)
    return (y * g.astype(jnp.float32)).astype(x.dtype)


def modulate(h, shift, scale):
    return h * (1 + scale) + shift


def heads(t, n_heads):
    return t.reshape(t.shape[:-1] + (n_heads, HEAD_DIM))


def short_conv(u, w):
    return lax.conv_general_dilated(u, w[:, None, :].astype(u.dtype), window_strides=(1,),
                                    padding=[(CONV_WIDTH // 2, CONV_WIDTH // 2)],
                                    dimension_numbers=('NWC', 'WIO', 'NWC'),
                                    feature_group_count=u.shape[-1])


def axial_rope_tables(n):
    t = jnp.arange(n)
    row = (t // GRID_W).astype(jnp.float32)
    col = (t % GRID_W).astype(jnp.float32)
    n_freq = HEAD_DIM // 4
    inv = ROPE_THETA ** (-jnp.arange(n_freq, dtype=jnp.float32) / n_freq)
    ang = jnp.stack([row[:, None] * inv, col[:, None] * inv], axis=1)
    return jnp.cos(ang), jnp.sin(ang)


def apply_axial_rope(x, cos, sin):
    xf = x.astype(jnp.float32).reshape(x.shape[:-1] + (2, 2, HEAD_DIM // 4))
    x1, x2 = xf[..., 0, :], xf[..., 1, :]
    cs, sn = cos[None, :, None], sin[None, :, None]
    out = jnp.stack([x1 * cs - x2 * sn, x2 * cs + x1 * sn], axis=-2)
    return out.reshape(x.shape).astype(x.dtype)


def softmax_with_sink(s, sink):
    m = jnp.maximum(jnp.max(s, axis=-1, keepdims=True), sink)
    p = jnp.exp(s - m)
    return p / (jnp.sum(p, axis=-1, keepdims=True) + jnp.exp(sink - m))


def neighbourhood_attention(q, k, v, kc, vc, rpb):
    bsz, n, h, dh = q.shape
    rows = n // GRID_W
    kr = min(NA_WIN_R, rows)
    nqb = GRID_W // NA_QB
    scale = dh ** -0.5
    qcol = np.arange(GRID_W).reshape(nqb, NA_QB)
    cstart = np.clip(qcol - NA_WIN_C // 2, 0, GRID_W - NA_WIN_C)
    kc0 = np.minimum(cstart[:, 0], GRID_W - NA_KB)
    kcol = kc0[:, None] + np.arange(NA_KB)
    col_valid = (kcol[:, None, :] >= cstart[..., None]) & (kcol[:, None, :] < cstart[..., None] + NA_WIN_C)
    dcol_idx = np.clip(kcol[:, None, :] - qcol[..., None] + NA_WIN_C - 1, 0, 2 * NA_WIN_C - 2)
    rpb_c = rpb.astype(jnp.float32)[:, :, dcol_idx]
    kg = k.reshape(bsz, rows, GRID_W, h, dh)
    vg = v.reshape(bsz, rows, GRID_W, h, dh)
    qrows = jnp.moveaxis(q.reshape(bsz, rows, nqb, NA_QB, h, dh), 1, 0)

    def one_row(args):
        qb, r = args
        rs = jnp.clip(r - NA_WIN_R // 2, 0, rows - kr)
        kb = lax.dynamic_slice_in_dim(kg, rs, kr, axis=1)[:, :, kcol]
        vb = lax.dynamic_slice_in_dim(vg, rs, kr, axis=1)[:, :, kcol]
        bias = jnp.take(rpb_c, rs + jnp.arange(kr) - r + NA_WIN_R - 1, axis=1)
        bias = bias.transpose(0, 2, 3, 1, 4)
        s_loc = jnp.einsum('bnqhd,brnkhd->bhnqrk', qb, kb, preferred_element_type=jnp.float32) * scale + bias
        s_loc = jnp.where(col_valid[:, :, None, :], s_loc, NEG_INF).reshape(bsz, h, nqb, NA_QB, kr * NA_KB)
        s_ctx = jnp.einsum('bnqhd,bchd->bhnqc', qb, kc, preferred_element_type=jnp.float32) * scale
        p = jax.nn.softmax(jnp.concatenate([s_loc, s_ctx], axis=-1), axis=-1).astype(v.dtype)
        p_loc = p[..., :kr * NA_KB].reshape(bsz, h, nqb, NA_QB, kr, NA_KB)
        out = (jnp.einsum('bhnqrk,brnkhd->bnqhd', p_loc, vb)
               + jnp.einsum('bhnqc,bchd->bnqhd', p[..., kr * NA_KB:], vc))
        return out.reshape(bsz, GRID_W, h * dh)

    out = lax.map(one_row, (qrows, jnp.arange(rows)))
    return jnp.moveaxis(out, 0, 1).reshape(bsz, n, h * dh)


def window_attention(q, k, v, kc, vc, sink):
    bsz, n, hq, dh = q.shape
    g = hq // SW_KV_HEADS
    nblk = n // SW_BLOCK
    span = SW_BLOCK + 2 * SW_RADIUS
    scale = dh ** -0.5
    kp = jnp.pad(k, ((0, 0), (SW_RADIUS, SW_RADIUS), (0, 0), (0, 0)))
    vp = jnp.pad(v, ((0, 0), (SW_RADIUS, SW_RADIUS), (0, 0), (0, 0)))
    rel = np.arange(span)[None, :] - SW_RADIUS - np.arange(SW_BLOCK)[:, None]
    band = np.abs(rel) <= SW_RADIUS
    sink_b = sink.astype(jnp.float32).reshape(SW_KV_HEADS, g)[None, :, :, None, None]
    qblk = jnp.moveaxis(q.reshape(bsz, nblk, SW_BLOCK, SW_KV_HEADS, g, dh), 1, 0)

    def one_block(args):
        qb, i = args
        s0 = i * SW_BLOCK
        kb = lax.dynamic_slice_in_dim(kp, s0, span, axis=1)
        vb = lax.dynamic_slice_in_dim(vp, s0, span, axis=1)
        kabs = s0 - SW_RADIUS + jnp.arange(span)
        valid = band & ((kabs >= 0) & (kabs < n))[None, :]
        s_loc = jnp.einsum('bqkgd,bskd->bkgqs', qb, kb, preferred_element_type=jnp.float32) * scale
        s_loc = jnp.where(valid, s_loc, NEG_INF)
        s_ctx = jnp.einsum('bqkgd,bckd->bkgqc', qb, kc, preferred_element_type=jnp.float32) * scale
        p = softmax_with_sink(jnp.concatenate([s_loc, s_ctx], axis=-1), sink_b).astype(v.dtype)
        out = (jnp.einsum('bkgqs,bskd->bqkgd', p[..., :span], vb)
               + jnp.einsum('bkgqc,bckd->bqkgd', p[..., span:], vc))
        return out.reshape(bsz, SW_BLOCK, hq * dh)

    out = lax.map(one_block, (qblk, jnp.arange(nblk)))
    return jnp.moveaxis(out, 0, 1).reshape(bsz, n, hq * dh)


def context_mh_attention(q, k, v):
    bsz, m, h, dh = q.shape
    s = jnp.einsum('bqhd,bkhd->bhqk', q, k, preferred_element_type=jnp.float32) * dh ** -0.5
    p = jax.nn.softmax(s, axis=-1).astype(v.dtype)
    return jnp.einsum('bhqk,bkhd->bqhd', p, v).reshape(bsz, m, h * dh)


def context_gqa_sink_attention(q, k, v, sink):
    bsz, m, hq, dh = q.shape
    g = hq // SW_KV_HEADS
    qg = q.reshape(bsz, m, SW_KV_HEADS, g, dh)
    s = jnp.einsum('bqkgd,bskd->bkgqs', qg, k, preferred_element_type=jnp.float32) * dh ** -0.5
    sink_b = sink.astype(jnp.float32).reshape(SW_KV_HEADS, g)[None, :, :, None, None]
    p = softmax_with_sink(s, sink_b).astype(v.dtype)
    return jnp.einsum('bkgqs,bskd->bqkgd', p, v).reshape(bsz, m, hq * dh)


def merge_groups(y_a, y_b, y_c, g):
    g_a, g_b, g_c = jnp.split(g, [CONV_CH, CONV_CH + NA_DIM])
    return jnp.concatenate([rms_norm(y_a, g_a), rms_norm(y_b, g_b), rms_norm(y_c, g_c)], axis=-1)


def swiglu(h, wg, wu, wd):
    return (jax.nn.silu(h @ wg) * (h @ wu)) @ wd


def moe_swiglu(h, w_router, wg, wu, wd):
    logits = jnp.einsum('btd,de->bte', h, w_router, preferred_element_type=jnp.float32)
    top_v, top_i = lax.top_k(logits, TOP_K)
    gates = jax.nn.softmax(top_v, axis=-1)
    combine = jnp.sum(jax.nn.one_hot(top_i, N_EXPERTS, dtype=jnp.float32) * gates[..., None], axis=-2)
    combine = combine.astype(h.dtype)
    out = jnp.zeros_like(h)
    for e in range(N_EXPERTS):
        out = out + combine[..., e:e + 1] * swiglu(h, wg[e], wu[e], wd[e])
    return out


def channel_mixer(h, l, ffn_w_gate, ffn_w_up, ffn_w_down, w_router, moe_w_gate, moe_w_up, moe_w_down):
    i = l // 2
    if l % 2 == 0:
        return swiglu(h, ffn_w_gate[i], ffn_w_up[i], ffn_w_down[i])
    return moe_swiglu(h, w_router[i], moe_w_gate[i], moe_w_up[i], moe_w_down[i])


def setup_inputs(seed: int = 0) -> dict:
    key = jax.random.key(seed)
    ks = iter(jax.random.split(key, 24))

    def nrm(shape, scale):
        return jax.random.normal(next(ks), shape, jnp.float32) * scale

    return {
        'x': nrm((BATCH, SEQ, D_MODEL), 1.0),
        'c': nrm((BATCH, D_MODEL), 1.0),
        'ctx': nrm((BATCH, CTX_LEN, D_MODEL), 1.0),
        'c_ctx': nrm((D_MODEL,), 1.0),
        'w_ada': nrm((DEPTH, D_MODEL, 6 * D_MODEL), 0.5 * D_MODEL ** -0.5),
        'b_ada': nrm((DEPTH, 6 * D_MODEL), 0.02),
        'g_norm1': 1.0 + nrm((DEPTH, D_MODEL), 0.02),
        'g_norm2': 1.0 + nrm((DEPTH, D_MODEL), 0.02),
        'w_in': nrm((DEPTH, D_MODEL, D_IN), D_MODEL ** -0.5),
        'conv_w': nrm((DEPTH, CONV_WIDTH, CONV_CH), CONV_WIDTH ** -0.5),
        'na_rpb': nrm((DEPTH, NA_HEADS, 2 * NA_WIN_R - 1, 2 * NA_WIN_C - 1), 0.1),
        'sw_sink': nrm((DEPTH, SW_HEADS), 0.5),
        'g_mix': 1.0 + nrm((DEPTH, D_MIX), 0.02),
        'w_out': nrm((DEPTH, D_MIX, D_MODEL), D_MIX ** -0.5),
        'ffn_w_gate': nrm((N_DENSE, D_MODEL, D_FF), D_MODEL ** -0.5),
        'ffn_w_up': nrm((N_DENSE, D_MODEL, D_FF), D_MODEL ** -0.5),
        'ffn_w_down': nrm((N_DENSE, D_FF, D_MODEL), D_FF ** -0.5),
        'w_router': nrm((N_MOE, D_MODEL, N_EXPERTS), D_MODEL ** -0.5),
        'moe_w_gate': nrm((N_MOE, N_EXPERTS, D_MODEL, D_FF_EXPERT), D_MODEL ** -0.5),
        'moe_w_up': nrm((N_MOE, N_EXPERTS, D_MODEL, D_FF_EXPERT), D_MODEL ** -0.5),
        'moe_w_down': nrm((N_MOE, N_EXPERTS, D_FF_EXPERT, D_MODEL), D_FF_EXPERT ** -0.5),
        'g_final': 1.0 + nrm((D_MODEL,), 0.02),
    }


def reference(x, c, ctx, c_ctx, w_ada, b_ada, g_norm1, g_norm2, w_in, conv_w, na_rpb, sw_sink, g_mix,
              w_out, ffn_w_gate, ffn_w_up, ffn_w_down, w_router, moe_w_gate, moe_w_up, moe_w_down, g_final):
    n = x.shape[1]
    cos, sin = axial_rope_tables(n)
    c_act = jax.nn.silu(c)
    cc_act = jax.nn.silu(c_ctx)
    xl, xc = x, ctx
    for l in range(DEPTH):
        last = l == DEPTH - 1
        sh1, sc1, g1, sh2, sc2, g2 = jnp.split((c_act @ w_ada[l] + b_ada[l])[:, None, :], 6, axis=-1)
        csh1, csc1, cg1, csh2, csc2, cg2 = jnp.split(cc_act @ w_ada[l] + b_ada[l], 6, axis=-1)

        hl = modulate(rms_norm(xl, g_norm1[l]), sh1, sc1)
        hc = modulate(rms_norm(xc, g_norm1[l]), csh1, csc1)
        pl = hl @ w_in[l]
        pc = hc @ (w_in[l][:, KV_OFF:] if last else w_in[l])
        a_h, a_b, a_c, na_q, sw_q = jnp.split(pl[..., :KV_OFF], Q_SPLITS, axis=-1)
        na_k, na_v, sw_k, sw_v = jnp.split(pl[..., KV_OFF:], KV_SPLITS, axis=-1)
        cna_k, cna_v, csw_k, csw_v = jnp.split(pc[..., -KV_WIDTH:], KV_SPLITS, axis=-1)
        cna_k, cna_v = heads(cna_k, NA_HEADS), heads(cna_v, NA_HEADS)
        csw_k, csw_v = heads(csw_k, SW_KV_HEADS), heads(csw_v, SW_KV_HEADS)

        y_a = a_b * short_conv(a_c * a_h, conv_w[l])
        y_b = neighbourhood_attention(heads(na_q, NA_HEADS), heads(na_k, NA_HEADS), heads(na_v, NA_HEADS),
                                      cna_k, cna_v, na_rpb[l])
        y_c = window_attention(apply_axial_rope(heads(sw_q, SW_HEADS), cos, sin),
                               apply_axial_rope(heads(sw_k, SW_KV_HEADS), cos, sin),
                               heads(sw_v, SW_KV_HEADS), csw_k, csw_v, sw_sink[l])
        mix_l = merge_groups(y_a, y_b, y_c, g_mix[l]) @ w_out[l]

        if not last:
            ca_h, ca_b, ca_c, cna_q, csw_q = jnp.split(pc[..., :KV_OFF], Q_SPLITS, axis=-1)
            yc_a = ca_b * short_conv(ca_c * ca_h, conv_w[l])
            yc_b = context_mh_attention(heads(cna_q, NA_HEADS), cna_k, cna_v)
            yc_c = context_gqa_sink_attention(heads(csw_q, SW_HEADS), csw_k, csw_v, sw_sink[l])
            xc = xc + cg1 * (merge_groups(yc_a, yc_b, yc_c, g_mix[l]) @ w_out[l])
        xl = xl + g1 * mix_l

        hl2 = modulate(rms_norm(xl, g_norm2[l]), sh2, sc2)
        xl = xl + g2 * channel_mixer(hl2, l, ffn_w_gate, ffn_w_up, ffn_w_down, w_router,
                                     moe_w_gate, moe_w_up, moe_w_down)
        if not last:
            hc2 = modulate(rms_norm(xc, g_norm2[l]), csh2, csc2)
            xc = xc + cg2 * channel_mixer(hc2, l, ffn_w_gate, ffn_w_up, ffn_w_down, w_router,
                                          moe_w_gate, moe_w_up, moe_w_down)
    return rms_norm(xl, g_final)
```

```python
import numpy as np
from contextlib import ExitStack
import concourse.bass as bass
import concourse.mybir as mybir
from concourse.bass_utils import run_bass_kernel_spmd

F32 = mybir.dt.float32
BF16 = mybir.dt.bfloat16
AF = mybir.ActivationFunctionType
ALU = mybir.AluOpType

D = 1024
SEQ = 4096
CTX = 256
NTOK = SEQ + CTX
DEPTH = 2
D_IN_EXT = 3072
D_FF = 2816
D_FFE = 3584
NE = 8
EPS = 1e-6

ENGS = ["pe", "act", "dve", "pool", "sp"]
NRING = 12


class Op:
    __slots__ = ("fn", "waits", "tok", "dma", "kind", "snap")

    def __init__(self, fn, dma):
        self.fn = fn
        self.waits = []
        self.tok = None
        self.dma = dma
        self.kind = None
        self.snap = None


class Prog:
    def __init__(self, nc):
        self.nc = nc
        self.ops = {e: [] for e in ENGS}
        self.cnt = {e: 0 for e in ENGS}
        self.dcnt = {e: 0 for e in ENGS}
        self.ringval = {e: [0] * NRING for e in ENGS}
        self.kw = {}
        self.kr = {}
        self.waited = {e: {} for e in ENGS}
        self.needed = set()

    def _wait(self, op, eng, sem, val):
        w = self.waited[eng]
        if w.get(sem, 0) >= val:
            return
        w[sem] = val
        op.waits.append((sem, val))
        if isinstance(sem, str):
            self.needed.add((sem, val))

    def emit(self, eng, fn, reads=(), writes=(), dma=False):
        op = Op(fn, dma)
        mysem = eng
        if dma:
            k = self.dcnt[eng]
            self.dcnt[eng] += 1
            r = k % NRING
            ring = (eng, r)
            pv = self.ringval[eng][r]
            if pv > 0:
                self._wait(op, eng, ring, pv)
            self.ringval[eng][r] = pv + 16
            tok = (ring, pv + 16)
        else:
            self.cnt[eng] += 1
            tok = (eng, self.cnt[eng])
        op.tok = tok
        for key in reads:
            for sem, val in self.kw.get(key, {}).items():
                self._wait(op, eng, sem, val)
        skip_same = (not dma) and (eng == "pe" or not STRICT)
        for key in writes:
            for sem, val in self.kw.get(key, {}).items():
                if sem == mysem and skip_same:
                    continue
                self._wait(op, eng, sem, val)
            for sem, val in self.kr.get(key, {}).items():
                if sem == mysem and skip_same:
                    continue
                self._wait(op, eng, sem, val)
        for key in reads:
            self.kr.setdefault(key, {})[tok[0]] = tok[1]
        for key in writes:
            self.kw.setdefault(key, {})[tok[0]] = tok[1]
            self.kr[key] = {}
        self.ops[eng].append(op)
        return op

    def _all_tokens(self):
        toks = []
        for e in ENGS:
            if self.cnt[e] > 0:
                toks.append((e, self.cnt[e]))
            for r in range(NRING):
                if self.ringval[e][r] > 0:
                    toks.append(((e, r), self.ringval[e][r]))
        return toks

    def barrier(self):
        toks = self._all_tokens()
        for e in ENGS:
            op = Op(None, False)
            for sem, val in toks:
                self._wait(op, e, sem, val)
            self.ops[e].append(op)

    def site(self, sidx, reads=()):
        for e in ENGS:
            op = Op(None, False)
            op.kind = ("site", sidx)
            op.snap = (self.cnt[e], list(self.ringval[e]))
            for key in reads:
                for sem, val in self.kw.get(key, {}).items():
                    self._wait(op, e, sem, val)
            self.ops[e].append(op)

    def end_sites(self):
        for e in ENGS:
            op = Op(None, False)
            op.kind = ("end",)
            op.snap = (self.cnt[e], list(self.ringval[e]))
            self.ops[e].append(op)
        self.waited = {e: {} for e in ENGS}

    def finish(self):
        op = Op(None, False)
        for sem, val in self._all_tokens():
            self._wait(op, "sp", sem, val)
        self.ops["sp"].append(op)

    def replay(self, stack):
        nc = self.nc
        rank = {}
        for e in ENGS:
            vals = sorted(v for (s, v) in self.needed if s == e)
            rank[e] = {v: i + 1 for i, v in enumerate(vals)}
        sems = {}
        for e in ENGS:
            sems[e] = stack.enter_context(nc.semaphore("s_" + e))
            for r in range(NRING):
                if self.ringval[e][r] > 0:
                    sems[(e, r)] = stack.enter_context(nc.semaphore("r_%s_%d" % (e, r)))
        block = stack.enter_context(nc.Block())

        import bisect
        nsorted = {e: sorted(rank[e].keys()) for e in ENGS}

        def run(e, eng):
            rk = rank[e]
            end_snap = None
            for op in self.ops[e]:
                if op.kind is not None and op.kind[0] == "end":
                    end_snap = op.snap
            ctx_stack = []
            loaded = False
            rank_of = lambda c: bisect.bisect_right(nsorted[e], c)
            for op in self.ops[e]:
                for sem, val in op.waits:
                    if isinstance(sem, str):
                        val = rank[sem][val]
                    eng.wait_ge(sems[sem], val)
                if op.kind is not None:
                    if op.kind[0] == "site":
                        if not loaded:
                            eng.reg_load(self.pred_regs[e], self.pred_ap)
                            loaded = True
                        cnt_s, ring_s = op.snap
                        cnt_e, ring_e = end_snap
                        cm = eng.If_lt(self.pred_regs[e], op.kind[1] + 1)
                        cm.__enter__()
                        r_s, r_e = rank_of(cnt_s), rank_of(cnt_e)
                        if r_s > 0:
                            eng.wait_ge(sems[e], r_s)
                        for r in range(NRING):
                            if ring_s[r] > 0:
                                eng.wait_ge(sems[(e, r)], ring_s[r])
                        if r_e > r_s:
                            eng.sem_inc(sems[e], r_e - r_s)
                        for r in range(NRING):
                            if ring_e[r] > ring_s[r]:
                                eng.sem_inc(sems[(e, r)], ring_e[r] - ring_s[r])
                        cm.__exit__(None, None, None)
                        cm2 = eng.Else()
                        cm2.__enter__()
                        ctx_stack.append(cm2)
                    else:
                        while ctx_stack:
                            ctx_stack.pop().__exit__(None, None, None)
                    continue
                if op.fn is None:
                    continue
                ins = op.fn(eng)
                if op.dma:
                    ins.then_inc(sems[op.tok[0]], 16)
                elif op.tok[1] in rk:
                    ins.then_inc(sems[e], 1)

        @block.tensor
        def _(eng):
            run("pe", eng)

        @block.scalar
        def _(eng):
            run("act", eng)

        @block.vector
        def _(eng):
            run("dve", eng)

        @block.gpsimd
        def _(eng):
            run("pool", eng)

        @block.sync
        def _(eng):
            run("sp", eng)


def _rs(r):
    return min(max(r - 4, 0), 56)


def na_plan():
    tables = []
    for dl in (-4, -2, 0, 2, 4):
        r0 = 20
        pat = tuple(tuple(1 if _rs(r0 + qr) <= r0 + dl + kr < _rs(r0 + qr) + 8 else 0 for qr in (0, 1)) for kr in (0, 1))
        tables.append((dl, pat))
    tiles = []
    for i in range(32):
        r0 = 2 * i
        lo = min(_rs(r0), _rs(r0 + 1))
        hi = max(_rs(r0), _rs(r0 + 1)) + 7
        lst = []
        for k0 in range((lo // 2) * 2, hi + 1, 2):
            pat = tuple(tuple(1 if _rs(r0 + qr) <= k0 + kr < _rs(r0 + qr) + 8 else 0 for qr in (0, 1)) for kr in (0, 1))
            if not any(any(p) for p in pat):
                continue
            key = (k0 - r0, pat)
            if key not in tables:
                tables.append(key)
            lst.append((k0, tables.index(key)))
        tiles.append(lst)
    return tiles, tables


NA_TILES, NA_TABLES = na_plan()
NT = len(NA_TABLES)


def na_table_indices():
    ridx = np.zeros((NT, 128, 128), np.int64)
    cidx = np.zeros((NT, 128, 128), np.int64)
    mask = np.zeros((NT, 128, 128), np.float32)
    kk = np.arange(128)
    kr, kc = kk // 64, kk % 64
    qr, qc = kk // 64, kk % 64
    cstart = np.clip(qc - 8, 0, 48)
    for t, (dl, pat) in enumerate(NA_TABLES):
        pat = np.array(pat)
        drow = dl + kr[:, None] - qr[None, :]
        rv = pat[kr[:, None], qr[None, :]]
        cv = (kc[:, None] >= cstart[None, :]) & (kc[:, None] < cstart[None, :] + 16)
        m = (rv == 1) & cv
        ridx[t] = np.where(m, np.clip(drow + 7, 0, 14), 0)
        cidx[t] = np.where(m, np.clip(kc[:, None] - qc[None, :] + 15, 0, 30), 0)
        mask[t] = m.astype(np.float32)
    return ridx, cidx, mask


def w_in_ext_cols():
    cols = list(range(0, 768))
    cols += list(range(768, 1152))
    swq0 = 1152
    perm_heads = [0, 3, 1, 4, 2, 5]
    swq = []
    swqs = []
    for h in perm_heads:
        base = swq0 + h * 64
        swq += list(range(base, base + 64))
        swqs += [base + (d + 16 if (d % 32) < 16 else d - 16) for d in range(64)]
    cols += swq + swqs
    cols += list(range(1536, 1920))
    swk0 = 2304
    swk = list(range(swk0, swk0 + 128))
    swks = []
    for h in range(2):
        base = swk0 + h * 64
        swks += [base + (d + 16 if (d % 32) < 16 else d - 16) for d in range(64)]
    cols += swk + swks
    cols += list(range(1920, 2304))
    cols += list(range(2432, 2560))
    assert len(cols) == D_IN_EXT
    return np.array(cols)


def rope_tables():
    t = np.arange(SEQ)
    row = (t // 64).astype(np.float32)
    col = (t % 64).astype(np.float32)
    inv = (10000.0 ** (-np.arange(16, dtype=np.float32) / 16)).astype(np.float32)
    C = np.zeros((64, SEQ), np.float32)
    S = np.zeros((64, SEQ), np.float32)
    for a, pos in enumerate((row, col)):
        ang = (pos[None, :] * inv[:, None]).astype(np.float32)
        c, s = np.cos(ang), np.sin(ang)
        C[a * 32:a * 32 + 16] = c
        C[a * 32 + 16:a * 32 + 32] = c
        S[a * 32:a * 32 + 16] = -s
        S[a * 32 + 16:a * 32 + 32] = s
    return np.concatenate([C, C], 0), np.concatenate([S, S], 0)


SB_LO = 16640
SB_HI = 229376


class Arena:
    def __init__(self, nc):
        self.nc = nc
        self.off = SB_LO
        self.n = 0

    def alloc(self, name, shape, dt):
        esz = 2 if dt == BF16 else 4
        nbytes = esz
        for s in shape[1:]:
            nbytes *= s
        nbytes = (nbytes + 63) // 64 * 64
        assert self.off + nbytes <= SB_HI, "SBUF overflow %s %d" % (name, self.off + nbytes - SB_HI)
        self.n += 1
        t = self.nc.alloc_sbuf_tensor_at("%s_%d" % (name, self.n), list(shape), dt, offset=self.off)
        self.off += nbytes
        return t

    def mark(self):
        return self.off

    def reset(self, m):
        self.off = m


SPARSE = True
SKIP = True
MC_CFG = 1152
STRICT = True


def build_nc(dbg=False, nlayers=DEPTH, stop_phase=99):
    nc = bass.Bass("TRN2", target_bir_lowering=False)
    I = "ExternalInput"
    xT_in = nc.dram_tensor("xT", [D, NTOK], F32, kind=I).ap()
    cv_in = nc.dram_tensor("cv", [128, 16], F32, kind=I).ap()
    wada_in = nc.dram_tensor("w_ada", [DEPTH, D, 6 * D], F32, kind=I).ap()
    bada_in = nc.dram_tensor("b_ada", [128, DEPTH * 48], F32, kind=I).ap()
    gn_in = nc.dram_tensor("gn", [128, DEPTH * 3 * 8 + 8], F32, kind=I).ap()
    win_in = nc.dram_tensor("w_in", [DEPTH, D, D_IN_EXT], F32, kind=I).ap()
    convw_in = nc.dram_tensor("convw", [128, DEPTH * 2 * 3], F32, kind=I).ap()
    rpbg_in = nc.dram_tensor("rpbg", [DEPTH, 6, 128, NT * 128], F32, kind=I).ap()
    sink_in = nc.dram_tensor("sink", [128, DEPTH * 6], F32, kind=I).ap()
    wout_in = nc.dram_tensor("w_out", [DEPTH, D, D], F32, kind=I).ap()
    fwg_in = nc.dram_tensor("ffn_wg", [D, D_FF], F32, kind=I).ap()
    fwu_in = nc.dram_tensor("ffn_wu", [D, D_FF], F32, kind=I).ap()
    fwd_in = nc.dram_tensor("ffn_wd", [D_FF, D], F32, kind=I).ap()
    wr_in = nc.dram_tensor("w_router", [128, 64], F32, kind=I).ap()
    if SPARSE:
        mwg_in = nc.dram_tensor("moe_wg", [NE * 14 * 128, 2048], F32, kind=I).ap()
        mwu_in = nc.dram_tensor("moe_wu", [NE * 14 * 128, 2048], F32, kind=I).ap()
        mwd_in = nc.dram_tensor("moe_wd", [NE * 4 * 128, 7 * D], F32, kind=I).ap()
    else:
        mwg_in = nc.dram_tensor("moe_wg", [NE, D, D_FFE], F32, kind=I).ap()
        mwu_in = nc.dram_tensor("moe_wu", [NE, D, D_FFE], F32, kind=I).ap()
        mwd_in = nc.dram_tensor("moe_wd", [NE, D_FFE, D], F32, kind=I).ap()
    CSTW = 128 * 3 + NT * 128 + 128 + 33 + 112 + 28
    cst_in = nc.dram_tensor("cst", [128, CSTW], F32, kind=I).ap()
    ropeC_in = nc.dram_tensor("ropeC", [128, SEQ], F32, kind=I).ap()
    ropeS_in = nc.dram_tensor("ropeS", [128, SEQ], F32, kind=I).ap()
    yT_out = nc.dram_tensor("yT", [D, SEQ], F32, kind="ExternalOutput").ap()
    skind = "ExternalOutput" if dbg else "Internal"
    xs = nc.dram_tensor("xs", [D, NTOK], F32, kind=skind).ap()
    qd = nc.dram_tensor("qd", [10, 128, NTOK], BF16, kind=skind).ap()
    dbgo = nc.dram_tensor("dbgo", [128, 4096], F32, kind=skind).ap() if dbg else None
    MC = MC_CFG
    MKS = MC // 128
    NSLOT = (2 * SEQ) // MC + NE
    MINU = -(-2 * SEQ // MC)
    I32 = mybir.dt.int32
    h2d = nc.dram_tensor("h2d", [SEQ + 128, D], BF16, kind="Internal").ap()
    macc = nc.dram_tensor("macc", [SEQ + 128, D], F32, kind=skind).ap()
    L2 = nc.dram_tensor("L2", [NSLOT * MC, 2], I32, kind=skind).ap()

    xT3 = xT_in.rearrange("(c p) t -> p c t", p=128)
    xs3 = xs.rearrange("(c p) t -> p c t", p=128)
    yT3 = yT_out.rearrange("(c p) t -> p c t", p=128)
    qd3 = qd.rearrange("k p t -> p k t")

    P = Prog(nc)
    A = Arena(nc)
    em = P.emit

    st = ExitStack()
    with st:
        psA = st.enter_context(nc.psum_tensor("psA", [128, 1024], F32))
        psB = st.enter_context(nc.psum_tensor("psB", [128, 1024], F32))
        ps4 = st.enter_context(nc.psum_tensor("ps4", [128, 512], F32))
        ps5 = st.enter_context(nc.psum_tensor("ps5", [128, 512], F32))
        ps6 = st.enter_context(nc.psum_tensor("ps6", [128, 1024], BF16))
        ps7 = st.enter_context(nc.psum_tensor("ps7", [128, 512], F32))
        PSH = [(psA, 0, "psA0"), (psA, 512, "psA1"), (psB, 0, "psB0"), (psB, 512, "psB1")]

        ident_f = A.alloc("ident_f", [128, 128], F32)
        ident_b = A.alloc("ident_b", [128, 128], BF16)
        ones_f = A.alloc("ones_f", [128, 128], F32)
        onesD = A.alloc("onesD", [128, 128], BF16)
        ones256 = A.alloc("ones256", [128, 128], BF16)
        maskLR = A.alloc("maskLR", [128, 256], BF16)
        maskNA = A.alloc("maskNA", [128, NT * 128], BF16)
        cact = A.alloc("cact", [128, 16], F32)
        adav = A.alloc("adav", [128, DEPTH, 48, 2], F32)
        Amod = A.alloc("Amod", [128, DEPTH, 2, 2, 8], F32)
        gn = A.alloc("gn", [128, DEPTH * 3 * 8 + 8], F32)
        bada = A.alloc("bada", [128, DEPTH * 48], F32)
        convw = A.alloc("convw", [128, DEPTH * 6], F32)
        esink = A.alloc("esink", [128, DEPTH * 6], F32)
        wr_sb = A.alloc("wr_sb", [128, 64], F32)
        ustrict = A.alloc("ustrict", [128, 128], F32)
        tokid = A.alloc("tokid", [128, 33], mybir.dt.int32)
        iotaGU = A.alloc("iotaGU", [128, 112], F32)
        iotaD = A.alloc("iotaD", [128, 28], F32)
        base_mark = A.mark()

        cstage = A.alloc("cstage", [128, CSTW], F32)
        em("sp", lambda e: e.dma_start(out=cstage[:], in_=cst_in), writes=["cstage"], dma=True)
        em("sp", lambda e: e.dma_start(out=cact[:], in_=cv_in), writes=["cact"], dma=True)
        em("sp", lambda e: e.dma_start(out=gn[:], in_=gn_in), writes=["gn"], dma=True)
        em("sp", lambda e: e.dma_start(out=bada[:], in_=bada_in), writes=["bada"], dma=True)
        em("sp", lambda e: e.dma_start(out=convw[:], in_=convw_in), writes=["convw"], dma=True)
        em("sp", lambda e: e.dma_start(out=esink[:], in_=sink_in), writes=["esink"], dma=True)
        em("sp", lambda e: e.dma_start(out=wr_sb[:], in_=wr_in), writes=["wr_sb"], dma=True)
        em("dve", lambda e: e.tensor_copy(out=ident_f[:], in_=cstage[:, 0:128]), reads=["cstage"], writes=["ident_f"])
        em("dve", lambda e: e.tensor_copy(out=ident_b[:], in_=cstage[:, 0:128]), reads=["cstage"], writes=["ident_b"])
        em("dve", lambda e: e.tensor_copy(out=maskLR[:], in_=cstage[:, 128:384]), reads=["cstage"], writes=["maskLR"])
        em("dve", lambda e: e.tensor_copy(out=maskNA[:], in_=cstage[:, 384:384 + NT * 128]), reads=["cstage"], writes=["maskNA"])
        em("dve", lambda e: e.tensor_copy(out=ustrict[:], in_=cstage[:, 384 + NT * 128:384 + NT * 128 + 128]), reads=["cstage"], writes=["ustrict"])
        em("dve", lambda e: e.tensor_copy(out=tokid[:], in_=cstage[:, 384 + NT * 128 + 128:384 + NT * 128 + 161]), reads=["cstage"], writes=["tokid"])
        em("dve", lambda e: e.tensor_copy(out=iotaGU[:], in_=cstage[:, 384 + NT * 128 + 161:384 + NT * 128 + 273]), reads=["cstage"], writes=["iotaGU"])
        em("dve", lambda e: e.tensor_copy(out=iotaD[:], in_=cstage[:, 384 + NT * 128 + 273:384 + NT * 128 + 301]), reads=["cstage"], writes=["iotaD"])
        em("pool", lambda e: e.memset(ones_f[:], 1.0), writes=["ones_f"])
        em("pool", lambda e: e.memset(onesD[:], 1.0 / D), writes=["onesD"])
        em("pool", lambda e: e.memset(ones256[:], 1.0 / 256), writes=["ones256"])
        em("act", lambda e: e.activation(out=esink[:], in_=esink[:], func=AF.Exp), reads=["esink"], writes=["esink"])
        em("act", lambda e: e.activation(out=cact[:], in_=cact[:], func=AF.Silu), reads=["cact"], writes=["cact"])

        wa = [A.alloc("wa%d" % i, [128, 8, 512], F32) for i in range(2)]
        cact3 = cact[:].rearrange("p (c w) -> p c w", w=2)
        for l in range(nlayers if stop_phase >= 0 else 0):
            wsrc = wada_in[l].rearrange("(c p) n -> p c n", p=128)
            for pc in range(12):
                s = pc % 2
                em("sp", lambda e, s=s, pc=pc, wsrc=wsrc: e.dma_start(out=wa[s][:], in_=wsrc[:, :, pc * 512:(pc + 1) * 512]),
                   writes=["wa%d" % s], dma=True)
                import os
                P0 = int(os.environ.get("P0", "9"))
                for nn in range(4 if P0 >= 1 else 0):
                    ch = pc * 4 + nn
                    for c in range(8):
                        em("pe", lambda e, s=s, nn=nn, c=c, ch=ch: e.matmul(
                            ps7[:, ch * 2:ch * 2 + 2], lhsT=wa[s][:, c, nn * 128:(nn + 1) * 128], rhs=cact3[:, c, :],
                            start=(c == 0), stop=(c == 7)), reads=["wa%d" % s, "cact"], writes=["ps7"])
            ps7v = ps7[:, 0:96].rearrange("p (k w) -> p k w", w=2)
            for w in range(2 if P0 >= 2 else 0):
                em("dve", lambda e, l=l, w=w, ps7v=ps7v: e.tensor_tensor(
                    out=adav[:, l, :, w], in0=ps7v[:, :, w], in1=bada[:, l * 48:(l + 1) * 48], op=ALU.add),
                    reads=["ps7", "bada"], writes=["adav"])
                for k in range(2 if P0 >= 3 else 0):
                    sc0 = 8 + 24 * k
                    em("dve", lambda e, l=l, w=w, k=k, sc0=sc0: e.scalar_tensor_tensor(
                        out=Amod[:, l, k, w, :], in0=adav[:, l, sc0:sc0 + 8, w], scalar=1.0,
                        in1=gn[:, (l * 3 + k) * 8:(l * 3 + k) * 8 + 8], op0=ALU.add, op1=ALU.mult),
                        reads=["adav", "gn"], writes=["Amod"])
        if dbg:
            em("sp", lambda e: e.dma_start(out=dbgo[:, 0:DEPTH * 96], in_=adav[:].rearrange("p l k w -> p (l k w)")), reads=["adav"], writes=["dbgo"], dma=True)
            em("sp", lambda e: e.dma_start(out=dbgo[:, 256:256 + DEPTH * 32], in_=Amod[:].rearrange("p l k w c -> p (l k w c)")), reads=["Amod"], writes=["dbgo"], dma=True)
        P.barrier()
        A.reset(base_mark)
        import os
        P1 = int(os.environ.get("P1", "9"))

        def modcols(l, k, w):
            a = lambda c: Amod[:, l, k, w, c:c + 1]
            b = lambda c: adav[:, l, 24 * k + c, w:w + 1]
            g = lambda c: adav[:, l, 24 * k + 16 + c, w:w + 1]
            return a, b, g

        def norm_mod(xt, xkey, TT, a, b, sq, sqkey, rstd, rkey, tmp2, tkeys, outs, okey, psbank, pskey):
            em("act", lambda e: e.activation(out=sq[:, :, 0:TT], in_=xt[:, :, 0:TT], func=AF.Square), reads=[xkey], writes=[sqkey])
            for c in range(8):
                em("pe", lambda e, c=c: e.matmul(psbank[:, 0:TT], lhsT=onesD[:], rhs=sq[:, c, 0:TT], start=(c == 0), stop=(c == 7)),
                   reads=[sqkey, "onesD"], writes=[pskey])
            em("act", lambda e: e.activation(out=rstd[:, 0:TT], in_=psbank[:, 0:TT], func=AF.Sqrt, bias=EPS, scale=1.0), reads=[pskey], writes=[rkey])
            em("dve", lambda e: e.reciprocal(out=rstd[:, 0:TT], in_=rstd[:, 0:TT]), reads=[rkey], writes=[rkey])
            for c in range(8):
                t = tmp2[c % 2]
                tk = tkeys[c % 2]
                em("dve", lambda e, c=c, t=t: e.scalar_tensor_tensor(out=t[:, 0:TT], in0=xt[:, c, 0:TT], scalar=a(c), in1=rstd[:, 0:TT],
                                                               op0=ALU.mult, op1=ALU.mult), reads=[xkey, rkey, "Amod"], writes=[tk])
                em("act", lambda e, c=c, t=t: e.activation(out=outs(c), in_=t[:, 0:TT], func=AF.Identity, bias=b(c), scale=1.0),
                   reads=[tk, "adav"], writes=[okey])


        def moe_sparse(l):
            a2, b2, g2 = modcols(l, 1, 0)
            NJ = D_FFE // 128
            mk0 = A.mark()
            Ui = A.alloc("Ui", [128, 1], I32)
            idxGU = A.alloc("idxGU", [128, NSLOT * 14], I32)
            idxD = A.alloc("idxD", [128, NSLOT * 4], I32)
            mk1_ = A.mark()
            idxtf = A.alloc("idxtf", [128, 112], F32)
            sel1 = A.alloc("sel1", [128, 32, 8], F32)
            sel2 = A.alloc("sel2", [128, 32, 8], F32)
            rank = A.alloc("rank", [128, 32, 8], F32)
            gate = A.alloc("gate", [128, 32, 2], F32)
            basec = A.alloc("basec", [128, 8], F32)
            rt = A.alloc("rt", [128, 96], F32)
            slots = A.alloc("slots", [128, 8], F32)
            cum = A.alloc("cum", [128, 8], F32)
            sbC = A.alloc("sbC", [128, 8], F32)
            posv = A.alloc("posv", [128, 32, 8], F32)
            posf = A.alloc("posf", [128, 32, 2], F32)
            posi2 = A.alloc("posi", [128, 64], I32)
            posi = posi2[:].rearrange("p (g k) -> p g k", k=2)
            entall2 = A.alloc("entall", [128, 128], I32)
            entall = entall2[:].rearrange("p (g k w) -> p g k w", k=2, w=2)
            eidf = A.alloc("eidf", [128, NSLOT], F32)
            eidi = A.alloc("eidi", [128, NSLOT], I32)
            fill = A.alloc("fill", [128, NSLOT * MKS, 2], I32)
            x3 = A.alloc("mx3", [128, 8, 512], F32)
            h32 = A.alloc("mh32", [128, 8, 512], F32)
            sq3 = A.alloc("msq3", [128, 8, 512], BF16)
            rstd3 = A.alloc("mrstd3", [128, 512], F32)
            tmp3 = [A.alloc("mtmp3", [128, 512], F32) for _ in range(2)]
            hTb = A.alloc("mhTb", [128, 8, 512], BF16)
            htok = [A.alloc("mhtok", [128, D], BF16) for _ in range(2)]
            zt = A.alloc("mzt", [128, D], F32)
            selt = A.alloc("mselt", [128, 8], F32)
            em("pool", lambda e: e.memset(zt[:], 0.0), writes=["mzt"])
            em("pool", lambda e: e.memset(basec[:], 0.0), writes=["basec"])
            em("pool", lambda e: e.memset(htok[0][:], 0.0), writes=["mhtok0"])
            em("sp", lambda e: e.dma_start(out=h2d[SEQ:SEQ + 128, :], in_=htok[0][:]), reads=["mhtok0"], writes=["h2d"], dma=True)
            for k in range(32):
                em("sp", lambda e, k=k: e.dma_start(out=macc[k * 128:(k + 1) * 128, :], in_=zt[:]), reads=["mzt"], writes=["maccz%d" % k], dma=True)
            em("sp", lambda e: e.dma_start(out=macc[SEQ:SEQ + 128, :], in_=zt[:]), reads=["mzt"], writes=["maccz32"], dma=True)
            em("dve", lambda e: e.tensor_copy(out=fill[:, :, 0], in_=tokid[:, 32:33].to_broadcast([128, NSLOT * MKS])), reads=["tokid"], writes=["fill"])
            em("pool", lambda e: e.memset(fill[:, :, 1:2], 0), writes=["fill"])
            em("sp", lambda e: e.dma_start(out=L2.rearrange("(k p) w -> p k w", p=128), in_=fill[:]), reads=["fill"], writes=["L2"], dma=True)
            h32_2 = [h32, A.alloc("mh32b", [128, 8, 512], F32)]
            hTb_2 = [hTb, A.alloc("mhTbb", [128, 8, 512], BF16)]

            def norm_part(ti):
                pb = ti % 2
                t0 = ti * 512
                h32_, hTb_ = h32_2[pb], hTb_2[pb]
                em("sp", lambda e: e.dma_start(out=x3[:], in_=xs3[:, :, t0:t0 + 512]), reads=["xs%d" % ti], writes=["mx3"], dma=True)
                norm_mod(x3, "mx3", 512, a2, b2, sq3, "msq3", rstd3, "mrstd3", tmp3, ["mtmp30", "mtmp31"],
                         lambda c: h32_[:, c, :], "mh32%d" % pb, ps4, "ps4")
                em("pool", lambda e: e.tensor_copy(out=hTb_[:], in_=h32_[:]), reads=["mh32%d" % pb], writes=["mhTb%d" % pb])

            norm_part(0)
            for ti in range(8):
                t0 = ti * 512
                if ti + 1 < 8:
                    norm_part(ti + 1)
                h32 = h32_2[ti % 2]
                hTb = hTb_2[ti % 2]
                hkey = "mh32%d" % (ti % 2)
                bkey = "mhTb%d" % (ti % 2)
                for sub in range(4):
                    gsub = ti * 4 + sub
                    hs = gsub % 2
                    for c in range(8):
                        em("pe", lambda e, c=c, sub=sub, hTb=hTb: e.transpose(out=ps6[:, c * 128:(c + 1) * 128], in_=hTb[:, c, sub * 128:(sub + 1) * 128], identity=ident_b[:]),
                           reads=[bkey, "ident_b"], writes=["ps6"])
                    em("act", lambda e, hs=hs: e.activation(out=htok[hs][:], in_=ps6[:], func=AF.Copy), reads=["ps6"], writes=["mhtok%d" % hs])
                    em("sp", lambda e, hs=hs, gsub=gsub: e.dma_start(out=h2d[gsub * 128:(gsub + 1) * 128, :], in_=htok[hs][:]),
                       reads=["mhtok%d" % hs], writes=["h2d%d" % gsub], dma=True)
                    for c in range(8):
                        em("pe", lambda e, c=c, sub=sub, h32=h32: e.matmul(ps7[:, 0:8], lhsT=h32[:, c, sub * 128:(sub + 1) * 128], rhs=wr_sb[:, c * 8:(c + 1) * 8],
                                                                start=(c == 0), stop=(c == 7)), reads=[hkey, "wr_sb"], writes=["ps7"])
                    lg = rt[:, 0:8]
                    m8 = rt[:, 8:16]
                    dd = rt[:, 32:33]
                    ga = gate[:, gsub, 0:1]
                    gb = gate[:, gsub, 1:2]
                    mk1 = sel1[:, gsub, :]
                    mk2 = sel2[:, gsub, :]
                    em("dve", lambda e, lg=lg: e.tensor_copy(out=lg, in_=ps7[:, 0:8]), reads=["ps7"], writes=["rt"])
                    em("dve", lambda e, lg=lg, m8=m8: e.max(out=m8, in_=lg), reads=["rt"], writes=["rt"])
                    em("dve", lambda e, lg=lg, m8=m8, mk1=mk1: e.tensor_scalar(out=mk1, in0=lg, scalar1=m8[:, 0:1], scalar2=None, op0=ALU.is_equal), reads=["rt"], writes=["sel"])
                    em("dve", lambda e, lg=lg, m8=m8, mk2=mk2: e.tensor_scalar(out=mk2, in0=lg, scalar1=m8[:, 1:2], scalar2=None, op0=ALU.is_equal), reads=["rt"], writes=["sel"])
                    em("dve", lambda e, m8=m8, dd=dd: e.tensor_tensor(out=dd, in0=m8[:, 1:2], in1=m8[:, 0:1], op=ALU.subtract), reads=["rt"], writes=["rt"])
                    em("act", lambda e, dd=dd: e.activation(out=dd, in_=dd, func=AF.Exp), reads=["rt"], writes=["rt"])
                    em("dve", lambda e, dd=dd, ga=ga: e.tensor_scalar(out=ga, in0=dd, scalar1=1.0, scalar2=None, op0=ALU.add), reads=["rt"], writes=["gate"])
                    em("dve", lambda e, ga=ga: e.reciprocal(out=ga, in_=ga), reads=["gate"], writes=["gate"])
                    em("dve", lambda e, dd=dd, ga=ga, gb=gb: e.tensor_tensor(out=gb, in0=dd, in1=ga, op=ALU.mult), reads=["rt", "gate"], writes=["gate"])
                    em("dve", lambda e, mk1=mk1, mk2=mk2: e.tensor_tensor(out=selt[:], in0=mk1, in1=mk2, op=ALU.add), reads=["sel"], writes=["mselt"])
                    em("pe", lambda e: e.matmul(ps7[:, 8:16], lhsT=ustrict[:], rhs=selt[:], start=True, stop=True), reads=["mselt", "ustrict"], writes=["ps7"])
                    em("pe", lambda e: e.matmul(ps7[:, 16:24], lhsT=ones_f[:], rhs=selt[:], start=True, stop=True), reads=["mselt", "ones_f"], writes=["ps7"])
                    em("dve", lambda e, gsub=gsub: e.tensor_tensor(out=rank[:, gsub, :], in0=ps7[:, 8:16], in1=basec[:], op=ALU.add), reads=["ps7", "basec"], writes=["rank"])
                    em("dve", lambda e: e.tensor_tensor(out=basec[:], in0=ps7[:, 16:24], in1=basec[:], op=ALU.add), reads=["ps7", "basec"], writes=["basec"])
            em("dve", lambda e: e.tensor_scalar(out=slots[:], in0=basec[:], scalar1=0.5, scalar2=None, op0=ALU.is_gt), reads=["basec"], writes=["slots"])
            for k in range(1, -(-SEQ // MC)):
                em("dve", lambda e, k=k: e.tensor_scalar(out=cum[:], in0=basec[:], scalar1=float(k * MC) + 0.5, scalar2=None, op0=ALU.is_gt), reads=["basec"], writes=["cum"])
                em("dve", lambda e: e.tensor_tensor(out=slots[:], in0=slots[:], in1=cum[:], op=ALU.add), reads=["slots", "cum"], writes=["slots"])
            em("dve", lambda e: e.tensor_copy(out=cum[:, 0:1], in_=slots[:, 0:1]), reads=["slots"], writes=["cum"])
            for ex in range(1, 8):
                em("dve", lambda e, ex=ex: e.tensor_tensor(out=cum[:, ex:ex + 1], in0=cum[:, ex - 1:ex], in1=slots[:, ex:ex + 1], op=ALU.add), reads=["cum", "slots"], writes=["cum"])
            em("dve", lambda e: e.tensor_tensor(out=sbC[:], in0=cum[:], in1=slots[:], op=ALU.subtract), reads=["cum", "slots"], writes=["sbC"])
            em("dve", lambda e: e.tensor_scalar(out=sbC[:], in0=sbC[:], scalar1=float(MC), scalar2=None, op0=ALU.mult), reads=["sbC"], writes=["sbC"])
            em("dve", lambda e: e.tensor_tensor(out=posv[:], in0=rank[:], in1=sbC[:].unsqueeze(1).to_broadcast([128, 32, 8]), op=ALU.add), reads=["rank", "sbC"], writes=["posv"])
            for k, sel in enumerate((sel1, sel2)):
                em("dve", lambda e, sel=sel: e.tensor_tensor(out=rank[:], in0=posv[:], in1=sel[:], op=ALU.mult), reads=["posv", "sel", "rank"], writes=["rank"])
                em("dve", lambda e, k=k: e.reduce_sum(out=posf[:, :, k], in_=rank[:], axis=mybir.AxisListType.X), reads=["rank"], writes=["posf"])
            em("dve", lambda e: e.tensor_copy(out=posi, in_=posf[:]), reads=["posf"], writes=["posi"])
            for k in range(2):
                em("dve", lambda e, k=k: e.tensor_copy(out=entall[:, :, k, 0], in_=tokid[:, 0:32]), reads=["tokid"], writes=["entall"])
                em("dve", lambda e, k=k: e.tensor_copy(out=entall[:, :, k, 1], in_=gate[:, :, k].bitcast(I32)), reads=["gate"], writes=["entall"])
            for sl_ in range(NSLOT):
                em("dve", lambda e, sl_=sl_: e.tensor_scalar(out=rt[:, 40:48], in0=cum[:], scalar1=float(sl_) + 0.5, scalar2=None, op0=ALU.is_lt), reads=["cum"], writes=["rt"])
                em("dve", lambda e, sl_=sl_: e.reduce_sum(out=eidf[:, sl_:sl_ + 1], in_=rt[:, 40:48], axis=mybir.AxisListType.X), reads=["rt"], writes=["eidf"])
            em("dve", lambda e: e.tensor_scalar(out=eidf[:], in0=eidf[:], scalar1=7.0, scalar2=None, op0=ALU.min), reads=["eidf"], writes=["eidf"])
            em("dve", lambda e: e.tensor_copy(out=eidi[:], in_=eidf[:]), reads=["eidf"], writes=["eidi"])
            em("dve", lambda e: e.tensor_copy(out=Ui[:], in_=cum[:, 7:8]), reads=["cum"], writes=["Ui"])
            em("dve", lambda e: e.tensor_scalar(out=rt[:, 48:48 + NSLOT], in0=eidf[:], scalar1=float(14 * 128), scalar2=None, op0=ALU.mult), reads=["eidf"], writes=["rt"])
            for sl_ in range(NSLOT):
                em("dve", lambda e, sl_=sl_: e.tensor_scalar(out=idxtf[:, 0:14], in0=iotaGU[:, 0:14], scalar1=rt[:, 48 + sl_:49 + sl_], scalar2=None, op0=ALU.add), reads=["rt", "iotaGU"], writes=["idxtf"])
                em("dve", lambda e, sl_=sl_: e.tensor_copy(out=idxGU[:, sl_ * 14:(sl_ + 1) * 14], in_=idxtf[:, 0:14]), reads=["idxtf"], writes=["idxGU"])
            em("dve", lambda e: e.tensor_scalar(out=rt[:, 48:48 + NSLOT], in0=eidf[:], scalar1=float(4 * 128), scalar2=None, op0=ALU.mult), reads=["eidf", "idxtf"], writes=["rt"])
            for sl_ in range(NSLOT):
                em("dve", lambda e, sl_=sl_: e.tensor_scalar(out=idxtf[:, 0:4], in0=iotaD[:, 0:4], scalar1=rt[:, 48 + sl_:49 + sl_], scalar2=None, op0=ALU.add), reads=["rt", "iotaD"], writes=["idxtf"])
                em("dve", lambda e, sl_=sl_: e.tensor_copy(out=idxD[:, sl_ * 4:(sl_ + 1) * 4], in_=idxtf[:, 0:4]), reads=["idxtf"], writes=["idxD"])
            for gsub in range(32):
                for k in range(2):
                    em("pool", lambda e, gsub=gsub, k=k: e.indirect_dma_start(
                        out=L2[:, :], out_offset=bass.IndirectOffsetOnAxis(ap=posi2[:, gsub * 2 + k:gsub * 2 + k + 1], axis=0), in_=entall2[:, (gsub * 2 + k) * 2:(gsub * 2 + k) * 2 + 2], in_offset=None,
                        ), reads=["posi", "entall", "L2"], writes=["L2s%d" % (gsub * 2 + k)], dma=True)
            if dbg:
                em("sp", lambda e: e.dma_start(out=dbgo[:, 1024:1032], in_=basec[:]), reads=["basec"], writes=["dbgo"], dma=True)
                em("sp", lambda e: e.dma_start(out=dbgo[:, 1032:1040], in_=cum[:]), reads=["cum"], writes=["dbgo"], dma=True)
                em("sp", lambda e: e.dma_start(out=dbgo[:, 1040:1040 + NSLOT], in_=eidf[:]), reads=["eidf"], writes=["dbgo"], dma=True)
            P.barrier()
            A.reset(mk1_)
            hTg = [A.alloc("hTg", [128, 8, MC], BF16) for _ in range(2)]
            inter = A.alloc("minter", [128, NJ, MC], BF16)
            wd_all = A.alloc("wd_all", [128, NJ, D], BF16)
            gu2 = [A.alloc("mgu", [128, 2, 8, 256], BF16) for _ in range(2)]
            hg3 = [A.alloc("hg", [128, D], BF16) for _ in range(3)]
            yst2 = [A.alloc("yst", [128, D], F32) for _ in range(2)]
            sg2 = [A.alloc("msg", [128, 384], BF16) for _ in range(2)]
            ent2 = [A.alloc("ent", [128, MKS * 2], I32) for _ in range(2)]
            items = [(sl_, jj) for sl_ in range(NSLOT) for jj in range(NJ // 2)]
            pfs = [0]

            def prefetch(upto):
                while pfs[0] < min(upto, len(items)):
                    sl_, jj = items[pfs[0]]
                    b = pfs[0] % 2
                    col = sl_ * 14 + jj
                    em("pool", lambda e, b=b, col=col: e.indirect_dma_start(out=gu2[b][:, 0].rearrange("p c n -> p (c n)"), out_offset=None, in_=mwg_in[:, :],
                                                                          in_offset=bass.IndirectOffsetOnAxis(ap=idxGU[:, col:col + 1], axis=0)),
                       reads=["idxGU"], writes=["mgu%d" % b], dma=True)
                    em("pool", lambda e, b=b, col=col: e.indirect_dma_start(out=gu2[b][:, 1].rearrange("p c n -> p (c n)"), out_offset=None, in_=mwu_in[:, :],
                                                                          in_offset=bass.IndirectOffsetOnAxis(ap=idxGU[:, col:col + 1], axis=0)),
                       reads=["idxGU"], writes=["mgu%d" % b], dma=True)
                    pfs[0] += 1

            def load_ent(sl_):
                b = sl_ % 2
                em("sp", lambda e: e.dma_start(out=ent2[b][:].rearrange("p (k w) -> p k w", w=2), in_=L2[sl_ * MC:(sl_ + 1) * MC, :].rearrange("(k p) w -> p k w", p=128)),
                   reads=["L2"] + ["L2s%d" % z for z in range(64)], writes=["ent%d" % b], dma=True)

            def gather_k(sl_, k):
                b = sl_ % 2
                hb = k % 3
                em("pool", lambda e: e.indirect_dma_start(out=hg3[hb][:, :], out_offset=None, in_=h2d[:, :],
                                                          in_offset=bass.IndirectOffsetOnAxis(ap=ent2[b][:, 2 * k:2 * k + 1], axis=0),
                                                          ), reads=["ent%d" % b, "h2d"] + ["h2d%d" % z for z in range(32)], writes=["hg%d" % hb], dma=True)

            def trans_k(sl_, k):
                b = sl_ % 2
                hb = k % 3
                for c in range(8):
                    em("pe", lambda e, c=c: e.transpose(out=ps6[:, c * 128:(c + 1) * 128], in_=hg3[hb][:, c * 128:(c + 1) * 128], identity=ident_b[:]),
                       reads=["hg%d" % hb, "ident_b"], writes=["ps6"])
                em("act", lambda e: e.activation(out=hTg[b][:, :, k * 128:(k + 1) * 128], in_=ps6[:].rearrange("p (c q) -> p c q", q=128), func=AF.Copy),
                   reads=["ps6"], writes=["hTg%d" % b])

            load_ent(0)
            for k in range(MKS):
                gather_k(0, k)
                trans_k(0, k)
            gi_ = [0]
            oi_ = [0]
            if SKIP:
                P.pred_ap = Ui[0:1, 0:1]
                P.pred_regs = {"pe": st.enter_context(nc.tensor.register("uU_pe")), "act": st.enter_context(nc.scalar.register("uU_act")),
                               "dve": st.enter_context(nc.vector.register("uU_dve")), "pool": st.enter_context(nc.gpsimd.register("uU_pool")),
                               "sp": st.enter_context(nc.sync.register("uU_sp"))}
            for sl_ in range(NSLOT):
                if SKIP and sl_ >= MINU:
                    P.site(sl_, reads=["Ui"])
                b = sl_ % 2
                hT_ = hTg[b]
                hk = "hTg%d" % b
                if sl_ + 1 < NSLOT:
                    load_ent(sl_ + 1)
                for q in range(4):
                    col = sl_ * 4 + q
                    em("pool", lambda e, q=q, col=col: e.indirect_dma_start(out=wd_all[:, q * 7:(q + 1) * 7, :].rearrange("p j n -> p (j n)"), out_offset=None, in_=mwd_in[:, :],
                                                                          in_offset=bass.IndirectOffsetOnAxis(ap=idxD[:, col:col + 1], axis=0)),
                       reads=["idxD"], writes=["wd_all"], dma=True)
                for jj in range(NJ // 2):
                    prefetch(sl_ * (NJ // 2) + jj + 2)
                    gb_ = (sl_ * (NJ // 2) + jj) % 2
                    if sl_ + 1 < NSLOT:
                        if 1 <= jj <= MKS:
                            gather_k(sl_ + 1, jj - 1)
                        if 2 <= jj <= MKS + 1:
                            trans_k(sl_ + 1, jj - 2)
                    for jh in range(2):
                        j = jj * 2 + jh
                        for tt in range(MC // 384):
                            gslot = gi_[0] % 2
                            gi_[0] += 1
                            gps, gpo, gpk = PSH[gslot]
                            ups, upo, upk = PSH[2 + gslot]
                            for c in range(8):
                                em("pe", lambda e, c=c, jh=jh, tt=tt, gps=gps, gpo=gpo, gb_=gb_, hT_=hT_: e.matmul(
                                    gps[:, gpo:gpo + 384], lhsT=gu2[gb_][:, 0, c, jh * 128:(jh + 1) * 128], rhs=hT_[:, c, tt * 384:(tt + 1) * 384],
                                    start=(c == 0), stop=(c == 7)), reads=["mgu%d" % gb_, hk], writes=[gpk])
                            for c in range(8):
                                em("pe", lambda e, c=c, jh=jh, tt=tt, ups=ups, upo=upo, gb_=gb_, hT_=hT_: e.matmul(
                                    ups[:, upo:upo + 384], lhsT=gu2[gb_][:, 1, c, jh * 128:(jh + 1) * 128], rhs=hT_[:, c, tt * 384:(tt + 1) * 384],
                                    start=(c == 0), stop=(c == 7)), reads=["mgu%d" % gb_, hk], writes=[upk])
                            sg = sg2[gslot]
                            em("act", lambda e, sg=sg, gps=gps, gpo=gpo: e.activation(out=sg[:], in_=gps[:, gpo:gpo + 384], func=AF.Silu),
                               reads=[gpk], writes=["msg%d" % gslot])
                            em("dve", lambda e, sg=sg, ups=ups, upo=upo, j=j, tt=tt: e.tensor_tensor(
                                out=inter[:, j, tt * 384:(tt + 1) * 384], in0=ups[:, upo:upo + 384], in1=sg[:], op=ALU.mult),
                                reads=[upk, "msg%d" % gslot], writes=["minter"])
                for k in range(MKS):
                    ys = oi_[0] % 2
                    oi_[0] += 1
                    yst = yst2[ys]
                    gcol = ent2[b][:, 2 * k + 1:2 * k + 2].bitcast(F32)
                    for half in range(2):
                        ops_, opk = (ps4, "ps4") if half == 0 else (ps5, "ps5")
                        for j in range(NJ):
                            em("pe", lambda e, j=j, k=k, half=half, ops_=ops_: e.matmul(
                                ops_[:, 0:512], lhsT=inter[:, j, k * 128:(k + 1) * 128], rhs=wd_all[:, j, half * 512:(half + 1) * 512],
                                start=(j == 0), stop=(j == NJ - 1)), reads=["minter", "wd_all"], writes=[opk])
                        em("dve", lambda e, yst=yst, ops_=ops_, half=half, gcol=gcol: e.tensor_scalar(
                            out=yst[:, half * 512:(half + 1) * 512], in0=ops_[:, 0:512], scalar1=gcol, scalar2=None, op0=ALU.mult),
                            reads=[opk, "ent%d" % b], writes=["yst%d" % ys])
                    em("pool", lambda e, yst=yst, k=k, b=b: e.indirect_dma_start(
                        out=macc[:, :], out_offset=bass.IndirectOffsetOnAxis(ap=ent2[b][:, 2 * k:2 * k + 1], axis=0), in_=yst[:, :], in_offset=None,
                        compute_op=ALU.add), reads=["yst%d" % ys, "ent%d" % b] + ["maccz%d" % z for z in range(33)], writes=["macc"], dma=True)
            if SKIP:
                P.end_sites()
            P.barrier()
            A.reset(mk0)

        def run_layer(l):
            last = l == DEPTH - 1
            xsrc3 = xT3 if l == 0 else xs3
            layer_mark = A.mark()
            kna = A.alloc("kna", [128, 3, NTOK], BF16)
            ksw = A.alloc("ksw", [128, NTOK], BF16)
            vna = A.alloc("vna", [128, 34, 6, 65], BF16)
            vsw = A.alloc("vsw", [128, 34, 2, 65], BF16)
            em("pool", lambda e, vna=vna: e.memset(vna[:, :, :, 64:65], 1.0), writes=["vna"])
            em("pool", lambda e, vsw=vsw: e.memset(vsw[:, :, :, 64:65], 1.0), writes=["vsw"])
            if stop_phase < 1:
                return
            p1_mark = A.mark()
            win = A.alloc("win", [128, 8, D_IN_EXT], BF16)
            wsrc = win_in[l].rearrange("(c p) n -> p c n", p=128)
            for c in range(8):
                em("pool", lambda e, c=c, wsrc=wsrc, win=win: e.dma_start(out=win[:, c, :], in_=wsrc[:, c, :]), writes=["win"], dma=True)
            xt2 = [A.alloc("xt", [128, 8, 512], F32)] * 2
            sq = A.alloc("sq", [128, 8, 512], BF16)
            hT2 = [A.alloc("hT", [128, 8, 512], BF16) for _ in range(2)]
            rstd = A.alloc("rstd", [128, 512], F32)
            tmp2 = [A.alloc("tmp", [128, 512], F32) for _ in range(2)]
            qst2 = [A.alloc("qst", [128, 10, 512], BF16)] * 2
            ah = A.alloc("ah", [128, 2, 512], F32)
            rC2 = [A.alloc("rC", [128, 512], F32) for _ in range(2)]
            rS2 = [A.alloc("rS", [128, 512], F32) for _ in range(2)]
            r1 = [A.alloc("r1", [128, 512], F32) for _ in range(2)]
            r2 = [A.alloc("r2", [128, 512], F32) for _ in range(2)]
            pfi = [0]

            def next_pf():
                i = pfi[0] % 3
                pfi[0] += 1
                return PSH[i]

            tiles = [(ti * 512, 512, 0) for ti in range(8)] + [(SEQ, CTX, 1)]
            def p1_norm(it):
                t0, TT, w = tiles[it]
                s = it % 2
                xt, hT = xt2[s], hT2[s]
                a, b, g = modcols(l, 0, w)
                em("sp", lambda e: e.dma_start(out=xt[:, :, 0:TT], in_=xsrc3[:, :, t0:t0 + TT]),
                   reads=["xs%d" % (t0 // 512)], writes=["xt"], dma=True)
                if w == 0:
                    em("sp", lambda e: e.dma_start(out=rC2[s][:], in_=ropeC_in[:, t0:t0 + 512]), writes=["rC%d" % s], dma=True)
                    em("sp", lambda e: e.dma_start(out=rS2[s][:], in_=ropeS_in[:, t0:t0 + 512]), writes=["rS%d" % s], dma=True)
                norm_mod(xt, "xt", TT, a, b, sq, "sq", rstd, "rstd", tmp2, ["tmp0", "tmp1"],
                         lambda c: hT[:, c, 0:TT], "hT%d" % s, ps7, "ps7")

            p1_norm(0)
            for it, (t0, TT, w) in enumerate(tiles):
                s = it % 2
                xt, hT, qst = xt2[s], hT2[s], qst2[s]
                xk, hk, qk = "xt", "hT%d" % s, "qst"
                rope = (w == 0)

                def proj(n, hT=hT, TT=TT, hk=hk):
                    pst, po, pk = next_pf()
                    for c in range(8):
                        em("pe", lambda e, c=c, pst=pst, po=po: e.matmul(pst[:, po:po + TT], lhsT=win[:, c, n * 128:(n + 1) * 128],
                                                                      rhs=hT[:, c, 0:TT], start=(c == 0), stop=(c == 7)),
                           reads=["win", hk], writes=[pk])
                    return pst[:, po:po + TT], pk

                if P1 < 1:
                    continue
                for n in (0, 1):
                    pa, pk = proj(n)
                    em("act", lambda e, pa=pa, n=n, TT=TT: e.activation(out=ah[:, n, 0:TT], in_=pa, func=AF.Copy), reads=[pk], writes=["ah"])
                for n in (2, 3):
                    pa, pk = proj(n)
                    em("act", lambda e, pa=pa, n=n, TT=TT, qst=qst: e.activation(out=qst[:, n, 0:TT], in_=pa, func=AF.Copy), reads=[pk], writes=[qk])
                for n in (4, 5):
                    pa, pk = proj(n)
                    em("dve", lambda e, pa=pa, n=n, TT=TT, qst=qst: e.tensor_tensor(out=qst[:, n - 4, 0:TT], in0=pa, in1=ah[:, n - 4, 0:TT], op=ALU.mult),
                       reads=[pk, "ah"], writes=[qk])
                if it + 1 < len(tiles):
                    p1_norm(it + 1)
                if P1 < 2:
                    continue
                for n in (6, 7, 8):
                    pa, pk = proj(n)
                    em("act", lambda e, pa=pa, n=n, TT=TT, qst=qst: e.activation(out=qst[:, n - 2, 0:TT], in_=pa, func=AF.Copy), reads=[pk], writes=[qk])
                for n in (15, 16, 17):
                    pa, pk = proj(n)
                    em("act", lambda e, pa=pa, n=n, TT=TT, t0=t0, kna=kna: e.activation(out=kna[:, n - 15, t0:t0 + TT], in_=pa, func=AF.Copy),
                       reads=[pk], writes=["kna"])

                def rope_out(n, ns, outap, okey, ri, s=s, rope=rope):
                    pa, pk = proj(n)
                    if not rope:
                        em("act", lambda e: e.activation(out=outap, in_=pa, func=AF.Copy), reads=[pk], writes=[okey])
                        return
                    pb, pkb = proj(ns)
                    i = ri % 2
                    em("dve", lambda e: e.tensor_tensor(out=r1[i][:], in0=pa, in1=rC2[s][:], op=ALU.mult), reads=[pk, "rC%d" % s], writes=["r1%d" % i])
                    em("dve", lambda e: e.tensor_tensor(out=r2[i][:], in0=pb, in1=rS2[s][:], op=ALU.mult), reads=[pkb, "rS%d" % s], writes=["r2%d" % i])
                    em("pool", lambda e: e.tensor_tensor(out=outap, in0=r1[i][:], in1=r2[i][:], op=ALU.add), reads=["r1%d" % i, "r2%d" % i], writes=[okey])

                if P1 < 3:
                    continue
                for j in range(3):
                    rope_out(9 + j, 12 + j, qst[:, 7 + j, 0:TT], qk, j)
                rope_out(18, 19, ksw[:, t0:t0 + TT], "ksw", 3)
                if P1 < 4:
                    continue
                em("sp", lambda e, qst=qst, t0=t0, TT=TT: e.dma_start(out=qd3[:, :, t0:t0 + TT], in_=qst[:, :, 0:TT]),
                   reads=[qk], writes=["qd%d" % (t0 // 512)], dma=True)
                for sub in range(TT // 128 if P1 >= 5 else 0):
                    pst, po, pk = next_pf()
                    for c in range(8):
                        em("pe", lambda e, c=c, pst=pst, po=po, sub=sub, hT=hT: e.matmul(
                            pst[:, po:po + 512], lhsT=hT[:, c, sub * 128:(sub + 1) * 128], rhs=win[:, c, 2560:3072],
                            start=(c == 0), stop=(c == 7)), reads=["win", hk], writes=[pk])
                    vch = t0 // 128 + sub
                    PV_ = int(os.environ.get("PV", "3"))
                    if PV_ >= 1:
                      em("act", lambda e, pst=pst, po=po, vch=vch, vna=vna: e.activation(
                        out=vna[:, vch, :, 0:64], in_=pst[:, po:po + 384].rearrange("p (h d) -> p h d", d=64), func=AF.Copy),
                        reads=[pk], writes=["vna"])
                    if PV_ >= 2:
                      em("act", lambda e, pst=pst, po=po, vch=vch, vsw=vsw: e.activation(
                        out=vsw[:, vch, :, 0:64], in_=pst[:, po + 384:po + 512].rearrange("p (h d) -> p h d", d=64), func=AF.Copy),
                        reads=[pk], writes=["vsw"])
            P.barrier()
            A.reset(p1_mark)
            if stop_phase < 2:
                return

            Eb = A.alloc("Eb", [128, 6, NT * 128], BF16)
            est = A.alloc("est", [128, NT * 128], F32)
            for h in range(6):
                em("sp", lambda e, h=h: e.dma_start(out=est[:], in_=rpbg_in[l, h]), writes=["est"], dma=True)
                em("act", lambda e: e.activation(out=est[:], in_=est[:], func=AF.Exp), reads=["est"], writes=["est"])
                em("dve", lambda e, h=h, Eb=Eb: e.tensor_tensor(out=Eb[:, h, :], in0=est[:], in1=maskNA[:], op=ALU.mult),
                   reads=["est", "maskNA"], writes=["Eb"])
            wout = A.alloc("wout", [128, 8, D], BF16)
            wsrc = wout_in[l].rearrange("(c p) n -> p c n", p=128)
            em("pool", lambda e, wsrc=wsrc, wout=wout: e.dma_start(out=wout[:], in_=wsrc), writes=["wout"], dma=True)
            for c in range(8):
                em("dve", lambda e, c=c, wout=wout: e.tensor_scalar(out=wout[:, c, :], in0=wout[:, c, :], scalar1=gn[:, (l * 3 + 2) * 8 + c:(l * 3 + 2) * 8 + c + 1],
                                                              scalar2=None, op0=ALU.mult), reads=["wout", "gn"], writes=["wout"])
            xt = A.alloc("xt", [128, 8, 512], F32)
            xo = A.alloc("xo", [128, 8, 512], F32)
            ut = A.alloc("ut", [128, 2, 514], BF16)
            abt = A.alloc("abt", [128, 2, 512], BF16)
            qn = A.alloc("qn", [128, 6, 512], BF16)
            cv1 = A.alloc("cv1", [128, 512], F32)
            ya = A.alloc("ya", [128, 2, 512], F32)
            sqa = A.alloc("sqa", [128, 2, 512], BF16)
            rsa = A.alloc("rsa", [128, 512], F32)
            yT = A.alloc("yT", [128, 8, 512], BF16)
            pt3 = [A.alloc("pt", [128, 896], BF16) for _ in range(3)]
            yg = [A.alloc("yg", [128, 6, 64], F32) for _ in range(2)]
            ygb = [A.alloc("ygb", [128, 384], BF16) for _ in range(2)]
            junk = A.alloc("junk", [128, 384], F32)
            st8 = A.alloc("st8", [128, 16], F32)
            pti = [0]
            spi = [0]

            def attn_A(qap, chunks, qkey):
                nchk = len(chunks)
                si = spi[0] % 2
                spi[0] += 1
                spt = psA if si == 0 else psB
                k0, k1 = ("psA0", "psA1") if si == 0 else ("psB0", "psB1")
                for ci, (kap, vap, m) in enumerate(chunks):
                    em("pe", lambda e, ci=ci, kap=kap: e.matmul(spt[:, ci * 128:(ci + 1) * 128], lhsT=kap, rhs=qap, start=True, stop=True),
                       reads=["kna", "ksw", qkey], writes=[k0 if ci < 4 else k1])
                pi = pti[0] % 3
                pti[0] += 1
                pt = pt3[pi]
                pk = "pt%d" % pi
                em("act", lambda e: e.activation(out=pt[:, 0:nchk * 128], in_=spt[:, 0:nchk * 128], func=AF.Exp, scale=0.125),
                   reads=[k0, k1], writes=[pk])
                ci = 0
                while ci < nchk:
                    m = chunks[ci][2]
                    if m is None:
                        ci += 1
                        continue
                    tbl, col0 = m
                    cj = ci + 1
                    while cj < nchk and chunks[cj][2] is not None and chunks[cj][2][0] is tbl and chunks[cj][2][1] == col0 + (cj - ci) * 128:
                        cj += 1
                    n = cj - ci
                    em("dve", lambda e, ci=ci, n=n, tbl=tbl, col0=col0: e.tensor_tensor(
                        out=pt[:, ci * 128:(ci + n) * 128], in0=pt[:, ci * 128:(ci + n) * 128], in1=tbl[col0:col0 + n * 128], op=ALU.mult),
                        reads=[pk, "Eb", "maskLR"], writes=[pk])
                    ci = cj
                return pt, pk

            def attn_B(ctx, chunks, ops_ap, opk, after):
                pt, pk = ctx
                nchk = len(chunks)
                for ci, (kap, vap, m) in enumerate(chunks):
                    em("pe", lambda e, ci=ci, vap=vap: e.matmul(ops_ap, lhsT=pt[:, ci * 128:(ci + 1) * 128], rhs=vap,
                                                             start=(ci == 0), stop=(ci == nchk - 1)),
                       reads=[pk, "vna", "vsw"], writes=[opk])
                if after is not None:
                    return after()
                return None

            class Tbl:
                def __init__(self, f):
                    self.f = f

                def __getitem__(self, sl):
                    return self.f(sl)

            EbT = [Tbl(lambda sl, h=h: Eb[:, h, sl]) for h in range(6)]
            mLR = Tbl(lambda sl: maskLR[:, sl])

            def finish_group(ops, opk, gi, sinkcols, ycols0, s):
                o3 = ops[:, 0:390].rearrange("p (h d) -> p h d", d=65)
                den = st8[:, gi * 8:gi * 8 + 6]
                if sinkcols is None:
                    em("dve", lambda e: e.tensor_copy(out=den, in_=o3[:, :, 64]), reads=[opk], writes=["st8"])
                else:
                    em("dve", lambda e: e.tensor_tensor(out=den, in0=o3[:, :, 64], in1=sinkcols, op=ALU.add), reads=[opk, "esink"], writes=["st8"])
                em("dve", lambda e: e.reciprocal(out=den, in_=den), reads=["st8"], writes=["st8"])
                y = yg[gi]
                yk = "yg%d" % gi
                em("dve", lambda e: e.tensor_tensor(out=y[:], in0=o3[:, :, 0:64], in1=den.unsqueeze(2).to_broadcast([128, 6, 64]), op=ALU.mult),
                   reads=[opk, "st8"], writes=[yk])
                y2 = y[:].rearrange("p h d -> p (h d)")
                ss = st8[:, gi * 8 + 6:gi * 8 + 7]
                em("dve", lambda e: e.memset(ss, 0.0), writes=["st8"])
                em("dve", lambda e: e.scalar_tensor_tensor(out=junk[:], in0=y2, scalar=1.0, in1=y2, op0=ALU.mult, op1=ALU.mult, accum_out=ss),
                   reads=[yk], writes=["junk", "st8"])
                em("act", lambda e: e.activation(out=ss, in_=ss, func=AF.Ln, scale=1.0 / 384, bias=EPS), reads=["st8"], writes=["st8"])
                em("act", lambda e: e.activation(out=ss, in_=ss, func=AF.Exp, scale=-0.5), reads=["st8"], writes=["st8"])
                yb = ygb[gi]
                ybk = "ygb%d" % gi
                em("dve", lambda e: e.tensor_scalar(out=yb[:], in0=y2, scalar1=ss, scalar2=None, op0=ALU.mult), reads=[yk, "st8"], writes=[ybk])

                def tail():
                    for j in range(3):
                        em("pe", lambda e, j=j: e.transpose(out=ps6[:, (gi * 3 + j) * 128:(gi * 3 + j + 1) * 128], in_=yb[:, j * 128:(j + 1) * 128], identity=ident_b[:]),
                           reads=[ybk, "ident_b"], writes=["ps6_%d" % gi])
                    em("act", lambda e: e.activation(out=yT[:, ycols0:ycols0 + 3, s * 128:(s + 1) * 128],
                                                     in_=ps6[:, gi * 384:(gi + 1) * 384].rearrange("p (j q) -> p j q", q=128), func=AF.Copy),
                       reads=["ps6_%d" % gi], writes=["yT"])
                return tail

            tiles2 = [(ti * 512, 512, 0) for ti in range(8)]
            if not last:
                tiles2.append((SEQ, CTX, 1))
            xt2 = [xt, xo]
            qn2 = [qn, A.alloc("qnb", [128, 6, 512], BF16)]
            ut2 = [ut, A.alloc("utb", [128, 2, 514], BF16)]
            abt2 = [abt, A.alloc("abtb", [128, 2, 512], BF16)]
            qdkeys = ["qd%d" % k for k in range(9)]

            def emit_loads(it):
                t0, TT, w = tiles2[it]
                sl = it % 2
                seq_lo, seq_hi = (0, SEQ) if w == 0 else (SEQ, NTOK)
                qn_, ut_, abt_ = qn2[sl], ut2[sl], abt2[sl]
                em("sp", lambda e: e.dma_start(out=qn_[:, :, 0:TT], in_=qd3[:, 4:10, t0:t0 + TT]), reads=qdkeys, writes=["qn%d" % sl], dma=True)
                lo = max(t0 - 1, seq_lo)
                hi = min(t0 + TT + 1, seq_hi)
                if lo > t0 - 1:
                    em("pool", lambda e: e.memset(ut_[:, :, 0:1], 0.0), writes=["ut%d" % sl])
                if hi < t0 + TT + 1:
                    em("pool", lambda e: e.memset(ut_[:, :, TT + 1:TT + 2], 0.0), writes=["ut%d" % sl])
                em("sp", lambda e: e.dma_start(out=ut_[:, :, lo - (t0 - 1):hi - (t0 - 1)], in_=qd3[:, 0:2, lo:hi]),
                   reads=qdkeys, writes=["ut%d" % sl], dma=True)
                em("sp", lambda e: e.dma_start(out=abt_[:, :, 0:TT], in_=qd3[:, 2:4, t0:t0 + TT]), reads=qdkeys, writes=["abt%d" % sl], dma=True)
                xt_ = xt2[sl]
                em("sp", lambda e: e.dma_start(out=xt_[:, :, 0:TT], in_=xsrc3[:, :, t0:t0 + TT]), reads=["xs%d" % (t0 // 512)], writes=["xt%d" % sl], dma=True)

            def emit_conv(it):
                t0, TT, w = tiles2[it]
                sl = it % 2
                ut_, abt_ = ut2[sl], abt2[sl]
                uk, ak = "ut%d" % sl, "abt%d" % sl
                for ch in range(2):
                    wc = lambda k, ch=ch: convw[:, l * 6 + ch * 3 + k:l * 6 + ch * 3 + k + 1]
                    em("dve", lambda e, ch=ch, wc=wc: e.tensor_scalar(out=cv1[:, 0:TT], in0=ut_[:, ch, 1:TT + 1], scalar1=wc(1), scalar2=None, op0=ALU.mult),
                       reads=[uk, "convw"], writes=["cv1"])
                    em("dve", lambda e, ch=ch, wc=wc: e.scalar_tensor_tensor(out=cv1[:, 0:TT], in0=ut_[:, ch, 0:TT], scalar=wc(0), in1=cv1[:, 0:TT],
                                                                       op0=ALU.mult, op1=ALU.add), reads=[uk, "convw", "cv1"], writes=["cv1"])
                    em("dve", lambda e, ch=ch, wc=wc: e.scalar_tensor_tensor(out=cv1[:, 0:TT], in0=ut_[:, ch, 2:TT + 2], scalar=wc(2), in1=cv1[:, 0:TT],
                                                                       op0=ALU.mult, op1=ALU.add), reads=[uk, "convw", "cv1"], writes=["cv1"])
                    em("dve", lambda e, ch=ch: e.tensor_tensor(out=ya[:, ch, 0:TT], in0=cv1[:, 0:TT], in1=abt_[:, ch, 0:TT], op=ALU.mult),
                       reads=["cv1", ak], writes=["ya"])
                em("pool", lambda e: e.tensor_tensor(out=sqa[:, :, 0:TT], in0=ya[:, :, 0:TT], in1=ya[:, :, 0:TT], op=ALU.mult), reads=["ya"], writes=["sqa"])
                for ch in range(2):
                    em("pe", lambda e, ch=ch: e.matmul(ps7[:, 0:TT], lhsT=ones256[:], rhs=sqa[:, ch, 0:TT], start=(ch == 0), stop=(ch == 1)),
                       reads=["sqa", "ones256"], writes=["ps7"])
                em("act", lambda e: e.activation(out=rsa[:, 0:TT], in_=ps7[:, 0:TT], func=AF.Ln, bias=EPS, scale=1.0), reads=["ps7"], writes=["rsa"])
                em("act", lambda e: e.activation(out=rsa[:, 0:TT], in_=rsa[:, 0:TT], func=AF.Exp, scale=-0.5), reads=["rsa"], writes=["rsa"])
                for ch in range(2):
                    em("dve", lambda e, ch=ch: e.tensor_tensor(out=yT[:, ch, 0:TT], in0=ya[:, ch, 0:TT], in1=rsa[:, 0:TT], op=ALU.mult),
                       reads=["ya", "rsa"], writes=["yT"])

            emit_loads(0)
            for it, (t0, TT, w) in enumerate(tiles2):
                a1, b1, g1 = modcols(l, 0, w)
                xkey = "xs%d" % (t0 // 512)
                qn = qn2[it % 2]
                qkey = "qn%d" % (it % 2)
                tasks = []
                for s in range(TT // 128):
                    blk = t0 // 128 + s
                    ctxk = [(SEQ + cc * 128, 32 + cc) for cc in range(2)]
                    for h in range(6):
                        j, po = h // 2, (h % 2) * 64
                        qap = qn[po:po + 64, j, s * 128:(s + 1) * 128]
                        chunks = []
                        if w == 0:
                            for (k0r, tb) in NA_TILES[blk]:
                                chunks.append((kna[po:po + 64, j, k0r * 64:k0r * 64 + 128], vna[:, k0r // 2, h, :], (EbT[h], tb * 128)))
                        for (kc0, vch) in ctxk:
                            chunks.append((kna[po:po + 64, j, kc0:kc0 + 128], vna[:, vch, h, :], None))
                        tasks.append((qap, chunks, ps4[:, h * 65:(h + 1) * 65], "ps4",
                                      (lambda s=s: finish_group(ps4, "ps4", 0, None, 2, s)) if h == 5 else None))
                    for h in range(6):
                        gq, po = h // 3, (h // 3) * 64
                        qap = qn[po:po + 64, 3 + h % 3, s * 128:(s + 1) * 128]
                        chunks = []
                        if w == 0:
                            if blk > 0:
                                chunks.append((ksw[po:po + 64, (blk - 1) * 128:blk * 128], vsw[:, blk - 1, gq, :], (mLR, 0)))
                            chunks.append((ksw[po:po + 64, blk * 128:(blk + 1) * 128], vsw[:, blk, gq, :], None))
                            if blk < 31:
                                chunks.append((ksw[po:po + 64, (blk + 1) * 128:(blk + 2) * 128], vsw[:, blk + 1, gq, :], (mLR, 128)))
                        for (kc0, vch) in ctxk:
                            chunks.append((ksw[po:po + 64, kc0:kc0 + 128], vsw[:, vch, gq, :], None))
                        tasks.append((qap, chunks, ps5[:, h * 65:(h + 1) * 65], "ps5",
                                      (lambda s=s: finish_group(ps5, "ps5", 1, esink[:, l * 6:l * 6 + 6], 5, s)) if h == 5 else None))
                LA = 2
                DEFER = 3
                ctxs = {}
                tails = []
                for i in range(len(tasks) + LA):
                    if i < len(tasks):
                        ctxs[i] = attn_A(tasks[i][0], tasks[i][1], qkey)
                    if i - LA >= 0:
                        tk = tasks[i - LA]
                        tl = attn_B(ctxs.pop(i - LA), tk[1], tk[2], tk[3], tk[4])
                        if tl is not None:
                            tails.append((i - LA + DEFER, tl))
                        while tails and tails[0][0] <= i - LA:
                            tails.pop(0)[1]()
                    if i == 8 and it + 1 < len(tiles2):
                        emit_loads(it + 1)
                    if i == 4:
                        emit_conv(it)
                if len(tasks) <= 8 and it + 1 < len(tiles2):
                    emit_loads(it + 1)
                for _, tl in tails:
                    tl()
                for m in range(8):
                    for c in range(8):
                        em("pe", lambda e, m=m, c=c, TT=TT: e.matmul(ps7[:, 0:TT], lhsT=wout[:, c, m * 128:(m + 1) * 128], rhs=yT[:, c, 0:TT],
                                                                  start=(c == 0), stop=(c == 7)), reads=["wout", "yT"], writes=["ps7"])
                    em("dve", lambda e, m=m, TT=TT, g1=g1, xt_=xt2[it % 2]: e.scalar_tensor_tensor(out=xt_[:, m, 0:TT], in0=ps7[:, 0:TT], scalar=g1(m), in1=xt_[:, m, 0:TT],
                                                                           op0=ALU.mult, op1=ALU.add), reads=["ps7", "xt%d" % (it % 2), "adav"], writes=["xt%d" % (it % 2)])
                em("sp", lambda e, t0=t0, TT=TT, xt_=xt2[it % 2]: e.dma_start(out=xs3[:, :, t0:t0 + TT], in_=xt_[:, :, 0:TT]), reads=["xt%d" % (it % 2)], writes=[xkey], dma=True)
            P.barrier()
            A.reset(layer_mark)
            if stop_phase < 3:
                return

            moe = (l % 2 == 1)
            if moe and SPARSE:
                moe_sparse(l)
                return
            NJ = (D_FFE if moe else D_FF) // 128
            nexp = NE if moe else 1
            TS = 2048
            hT = A.alloc("hT3", [128, 8, TS], BF16)
            m_inter = A.mark()
            inter = A.alloc("inter", [128, NJ, TS], BF16)
            combbc = A.alloc("combbc", [128, TS], F32)
            comb = A.alloc("comb", [128, 16, 8], F32)
            gu2 = [A.alloc("gu", [128, 2, 8, 256], BF16) for _ in range(2)]
            wd2 = [A.alloc("wd", [128, NJ, 128], BF16) for _ in range(2)]
            stg3 = [A.alloc("stg", [128, 512], F32) for _ in range(3)]
            sg2 = [A.alloc("sg", [128, 512], BF16) for _ in range(2)]
            diag = A.alloc("diag", [128, 128], F32)
            rt = A.alloc("rt", [128, 64], F32)
            p3_mark = A.mark()
            A.reset(m_inter)
            x3 = A.alloc("xt3", [128, 8, 512], F32)
            h32 = A.alloc("h32", [128, 8, 512], F32)
            sq3v = A.alloc("sq3", [128, 8, 512], BF16)
            rstd3v = A.alloc("rstd3", [128, 512], F32)
            tmp3v = [A.alloc("tmp3", [128, 512], F32) for _ in range(2)]
            A.reset(p3_mark)

            if moe:
                wg_e = lambda ex: mwg_in[ex]
                wu_e = lambda ex: mwu_in[ex]
                wd_e = lambda ex: mwd_in[ex]
            else:
                wg_e = lambda ex: fwg_in
                wu_e = lambda ex: fwu_in
                wd_e = lambda ex: fwd_in

            supers = [(0, TS, 0), (TS, TS, 0)]
            if not last:
                supers.append((SEQ, CTX, 1))
            items = []
            for si, (s0, T, w) in enumerate(supers):
                for ex in range(nexp):
                    for jj in range(NJ // 2):
                        items.append(("gu", si, ex, jj))
                    for m in range(8):
                        items.append(("wd", si, ex, m))
            cnt = {"gu": 0, "wd": 0}
            slot_of = {}
            for itx in items:
                slot_of[itx] = cnt[itx[0]] % 2
                cnt[itx[0]] += 1
            pf_state = [0]

            def prefetch(upto):
                while pf_state[0] < min(upto, len(items)):
                    kind, si, ex, idx = items[pf_state[0]]
                    sl = slot_of[items[pf_state[0]]]
                    if kind == "gu":
                        srcg = wg_e(ex).rearrange("(c p) n -> p c n", p=128)[:, :, idx * 256:(idx + 1) * 256]
                        srcu = wu_e(ex).rearrange("(c p) n -> p c n", p=128)[:, :, idx * 256:(idx + 1) * 256]
                        em("pool", lambda e, sl=sl, srcg=srcg: e.dma_start(out=gu2[sl][:, 0, :, :], in_=srcg), writes=["gu%d" % sl], dma=True)
                        em("pool", lambda e, sl=sl, srcu=srcu: e.dma_start(out=gu2[sl][:, 1, :, :], in_=srcu), writes=["gu%d" % sl], dma=True)
                    else:
                        src = wd_e(ex).rearrange("(j p) n -> p j n", p=128)[:, :, idx * 128:(idx + 1) * 128]
                        em("pool", lambda e, sl=sl, src=src: e.dma_start(out=wd2[sl][:], in_=src), writes=["wd%d" % sl], dma=True)
                    pf_state[0] += 1

            item_pos = {itx: i for i, itx in enumerate(items)}
            gi_ = [0]
            oi_ = [0]
            for si, (s0, T, w) in enumerate(supers):
                a2, b2, g2 = modcols(l, 1, w)
                ntile = max(T // 512, 1)
                TT = min(T, 512)
                P.barrier()
                for ti in range(ntile):
                    t0 = s0 + ti * TT
                    xkey = "xs%d" % (t0 // 512)
                    em("sp", lambda e, t0=t0, TT=TT: e.dma_start(out=x3[:, :, 0:TT], in_=xs3[:, :, t0:t0 + TT]), reads=[xkey], writes=["xt3"], dma=True)
                    if moe:
                        norm_mod(x3, "xt3", TT, a2, b2, sq3v, "sq3", rstd3v, "rstd3", tmp3v, ["tmp30", "tmp31"],
                                 lambda c, TT=TT: h32[:, c, 0:TT], "h32", ps7, "ps7")
                        em("pool", lambda e, ti=ti, TT=TT: e.tensor_copy(out=hT[:, :, ti * TT:(ti + 1) * TT], in_=h32[:, :, 0:TT]), reads=["h32"], writes=["hT3"])
                        for sub in range(TT // 128):
                            gsub = ti * 4 + sub
                            for c in range(8):
                                em("pe", lambda e, c=c, sub=sub: e.matmul(ps7[:, 0:8], lhsT=h32[:, c, sub * 128:(sub + 1) * 128], rhs=wr_sb[:, c * 8:(c + 1) * 8],
                                                                        start=(c == 0), stop=(c == 7)), reads=["h32", "wr_sb"], writes=["ps7"])
                            lg = rt[:, 0:8]
                            m8 = rt[:, 8:16]
                            mk1 = rt[:, 16:24]
                            mk2 = rt[:, 24:32]
                            dd = rt[:, 32:33]
                            ga = rt[:, 33:34]
                            gb = rt[:, 34:35]
                            em("dve", lambda e, lg=lg: e.tensor_copy(out=lg, in_=ps7[:, 0:8]), reads=["ps7"], writes=["rt"])
                            em("dve", lambda e, lg=lg, m8=m8: e.max(out=m8, in_=lg), reads=["rt"], writes=["rt"])
                            em("dve", lambda e, lg=lg, m8=m8, mk1=mk1: e.tensor_scalar(out=mk1, in0=lg, scalar1=m8[:, 0:1], scalar2=None, op0=ALU.is_equal), reads=["rt"], writes=["rt"])
                            em("dve", lambda e, lg=lg, m8=m8, mk2=mk2: e.tensor_scalar(out=mk2, in0=lg, scalar1=m8[:, 1:2], scalar2=None, op0=ALU.is_equal), reads=["rt"], writes=["rt"])
                            em("dve", lambda e, m8=m8, dd=dd: e.tensor_tensor(out=dd, in0=m8[:, 1:2], in1=m8[:, 0:1], op=ALU.subtract), reads=["rt"], writes=["rt"])
                            em("act", lambda e, dd=dd: e.activation(out=dd, in_=dd, func=AF.Exp), reads=["rt"], writes=["rt"])
                            em("dve", lambda e, dd=dd, ga=ga: e.tensor_scalar(out=ga, in0=dd, scalar1=1.0, scalar2=None, op0=ALU.add), reads=["rt"], writes=["rt"])
                            em("dve", lambda e, ga=ga: e.reciprocal(out=ga, in_=ga), reads=["rt"], writes=["rt"])
                            em("dve", lambda e, dd=dd, ga=ga, gb=gb: e.tensor_tensor(out=gb, in0=dd, in1=ga, op=ALU.mult), reads=["rt"], writes=["rt"])
                            em("dve", lambda e, mk1=mk1, ga=ga: e.tensor_scalar(out=mk1, in0=mk1, scalar1=ga, scalar2=None, op0=ALU.mult), reads=["rt"], writes=["rt"])
                            em("dve", lambda e, mk1=mk1, mk2=mk2, gb=gb, gsub=gsub: e.scalar_tensor_tensor(out=comb[:, gsub, :], in0=mk2, scalar=gb, in1=mk1, op0=ALU.mult, op1=ALU.add),
                               reads=["rt"], writes=["comb"])
                    else:
                        norm_mod(x3, "xt3", TT, a2, b2, sq3v, "sq3", rstd3v, "rstd3", tmp3v, ["tmp30", "tmp31"],
                                 lambda c, ti=ti, TT=TT: hT[:, c, ti * TT:(ti + 1) * TT], "hT3", ps7, "ps7")
                P.barrier()
                for ex in range(nexp):
                    if moe:
                        for sub in range(T // 128):
                            em("dve", lambda e, sub=sub, ex=ex: e.tensor_scalar(out=diag[:], in0=ident_f[:], scalar1=comb[:, sub, ex:ex + 1], scalar2=None, op0=ALU.mult),
                               reads=["ident_f", "comb"], writes=["diag"])
                            em("pe", lambda e, sub=sub: e.matmul(ps7[:, (sub % 4) * 128:(sub % 4 + 1) * 128], lhsT=ones_f[:], rhs=diag[:], start=True, stop=True),
                               reads=["diag", "ones_f"], writes=["ps7"])
                            if sub % 4 == 3:
                                em("act", lambda e, sub=sub: e.activation(out=combbc[:, (sub - 3) * 128:(sub + 1) * 128], in_=ps7[:, 0:512], func=AF.Copy),
                                   reads=["ps7"], writes=["combbc"])
                    for jj in range(NJ // 2):
                        itx = ("gu", si, ex, jj)
                        prefetch(item_pos[itx] + 2)
                        sl = slot_of[itx]
                        for jh in range(2):
                            j = jj * 2 + jh
                            for ti in range(ntile):
                                gslot = gi_[0] % 2
                                gi_[0] += 1
                                gps, gpo, gpk = PSH[gslot]
                                ups, upo, upk = PSH[2 + gslot]
                                for c in range(8):
                                    em("pe", lambda e, c=c, sl=sl, jh=jh, ti=ti, gps=gps, gpo=gpo, TT=TT: e.matmul(
                                        gps[:, gpo:gpo + TT], lhsT=gu2[sl][:, 0, c, jh * 128:(jh + 1) * 128], rhs=hT[:, c, ti * TT:(ti + 1) * TT],
                                        start=(c == 0), stop=(c == 7)), reads=["gu%d" % sl, "hT3"], writes=[gpk])
                                for c in range(8):
                                    em("pe", lambda e, c=c, sl=sl, jh=jh, ti=ti, ups=ups, upo=upo, TT=TT: e.matmul(
                                        ups[:, upo:upo + TT], lhsT=gu2[sl][:, 1, c, jh * 128:(jh + 1) * 128], rhs=hT[:, c, ti * TT:(ti + 1) * TT],
                                        start=(c == 0), stop=(c == 7)), reads=["gu%d" % sl, "hT3"], writes=[upk])
                                sg = sg2[gslot]
                                em("act", lambda e, sg=sg, gps=gps, gpo=gpo, TT=TT: e.activation(out=sg[:, 0:TT], in_=gps[:, gpo:gpo + TT], func=AF.Silu),
                                   reads=[gpk], writes=["sg%d" % gslot])
                                em("dve", lambda e, sg=sg, ups=ups, upo=upo, j=j, ti=ti, TT=TT: e.tensor_tensor(
                                    out=inter[:, j, ti * TT:(ti + 1) * TT], in0=ups[:, upo:upo + TT], in1=sg[:, 0:TT], op=ALU.mult),
                                    reads=[upk, "sg%d" % gslot], writes=["inter"])
                    for m in range(8):
                        itx = ("wd", si, ex, m)
                        prefetch(item_pos[itx] + 2)
                        sl = slot_of[itx]
                        for ti in range(ntile):
                            t0 = s0 + ti * TT
                            xkey = "xs%d" % (t0 // 512)
                            oslot = oi_[0] % 2
                            so = oi_[0] % 3
                            oi_[0] += 1
                            ops_, opk = (ps4, "ps4") if oslot == 0 else (ps5, "ps5")
                            for j in range(NJ):
                                em("pe", lambda e, j=j, sl=sl, ti=ti, ops_=ops_, TT=TT: e.matmul(
                                    ops_[:, 0:TT], lhsT=wd2[sl][:, j, :], rhs=inter[:, j, ti * TT:(ti + 1) * TT], start=(j == 0), stop=(j == NJ - 1)),
                                    reads=["wd%d" % sl, "inter"], writes=[opk])
                            stg = stg3[so]
                            if moe:
                                em("dve", lambda e, stg=stg, ops_=ops_, m=m, ti=ti, TT=TT, g2=g2: e.scalar_tensor_tensor(
                                    out=stg[:, 0:TT], in0=ops_[:, 0:TT], scalar=g2(m), in1=combbc[:, ti * TT:(ti + 1) * TT], op0=ALU.mult, op1=ALU.mult),
                                    reads=[opk, "combbc", "adav"], writes=["stg%d" % so])
                            else:
                                em("dve", lambda e, stg=stg, ops_=ops_, m=m, TT=TT, g2=g2: e.tensor_scalar(
                                    out=stg[:, 0:TT], in0=ops_[:, 0:TT], scalar1=g2(m), scalar2=None, op0=ALU.mult),
                                    reads=[opk, "adav"], writes=["stg%d" % so])
                            em("pool", lambda e, stg=stg, m=m, t0=t0, TT=TT: e.dma_start(out=xs3[:, m, t0:t0 + TT], in_=stg[:, 0:TT], accum_op=ALU.add),
                               reads=["stg%d" % so], writes=[xkey], dma=True)
            P.barrier()
            A.reset(layer_mark)

        for l_ in range(nlayers):
            run_layer(l_)

        if stop_phase >= 4:
            xt2 = [A.alloc("xtf", [128, 8, 512], F32) for _ in range(2)]
            sq = A.alloc("sqf", [128, 8, 512], BF16)
            rstd = A.alloc("rstdf", [128, 512], F32)
            yo2 = [A.alloc("yof", [128, 8, 512], F32) for _ in range(2)]
            mt2 = [A.alloc("mtf", [128, D], F32) for _ in range(2)]
            gf0 = DEPTH * 3 * 8
            for ti in range(8):
                s = ti % 2
                xt, yo = xt2[s], yo2[s]
                t0 = ti * 512
                em("sp", lambda e, xt=xt, t0=t0: e.dma_start(out=xt[:], in_=xs3[:, :, t0:t0 + 512]), reads=["xs%d" % ti], writes=["xtf%d" % s], dma=True)
                if SPARSE and nlayers == DEPTH:
                    _, _, g2f = modcols(DEPTH - 1, 1, 0)
                    for sub in range(4):
                        mi = (ti * 4 + sub) % 2
                        mt = mt2[mi]
                        pX, pk0, pk1 = (psA, "psA0", "psA1") if mi == 0 else (psB, "psB0", "psB1")
                        em("sp", lambda e, mt=mt, t0=t0, sub=sub: e.dma_start(out=mt[:], in_=macc[t0 + sub * 128:t0 + (sub + 1) * 128, :]),
                           reads=["macc"], writes=["mt%d" % mi], dma=True)
                        for c in range(8):
                            em("pe", lambda e, c=c, mt=mt, pX=pX: e.transpose(out=pX[:, c * 128:(c + 1) * 128], in_=mt[:, c * 128:(c + 1) * 128], identity=ident_f[:]),
                               reads=["mt%d" % mi, "ident_f"], writes=[pk0 if c < 4 else pk1])
                        for c in range(8):
                            em("dve", lambda e, c=c, xt=xt, pX=pX, sub=sub, g2f=g2f: e.scalar_tensor_tensor(
                                out=xt[:, c, sub * 128:(sub + 1) * 128], in0=pX[:, c * 128:(c + 1) * 128], scalar=g2f(c), in1=xt[:, c, sub * 128:(sub + 1) * 128],
                                op0=ALU.mult, op1=ALU.add), reads=[pk0 if c < 4 else pk1, "xtf%d" % s, "adav"], writes=["xtf%d" % s])
                em("act", lambda e, xt=xt: e.activation(out=sq[:], in_=xt[:], func=AF.Square), reads=["xtf%d" % s], writes=["sqf"])
                for c in range(8):
                    em("pe", lambda e, c=c: e.matmul(ps7[:], lhsT=onesD[:], rhs=sq[:, c, :], start=(c == 0), stop=(c == 7)), reads=["sqf", "onesD"], writes=["ps7"])
                em("act", lambda e: e.activation(out=rstd[:], in_=ps7[:], func=AF.Sqrt, bias=EPS, scale=1.0), reads=["ps7"], writes=["rstdf"])
                em("dve", lambda e: e.reciprocal(out=rstd[:], in_=rstd[:]), reads=["rstdf"], writes=["rstdf"])
                for c in range(8):
                    em("dve", lambda e, c=c, xt=xt, yo=yo: e.scalar_tensor_tensor(out=yo[:, c, :], in0=xt[:, c, :], scalar=gn[:, gf0 + c:gf0 + c + 1], in1=rstd[:],
                                                                           op0=ALU.mult, op1=ALU.mult), reads=["xtf%d" % s, "rstdf", "gn"], writes=["yof%d" % s])
                em("sp", lambda e, yo=yo, t0=t0: e.dma_start(out=yT3[:, :, t0:t0 + 512], in_=yo[:]), reads=["yof%d" % s], writes=["yout"], dma=True)
        P.finish()
        P.replay(st)
    return nc


def _pcols(v):
    v = np.asarray(v, np.float32)
    lead = v.shape[:-1]
    n = v.shape[-1] // 128
    v = v.reshape(lead + (n, 128))
    return np.ascontiguousarray(np.moveaxis(v, -1, 0))


def prep_shared(inputs):
    f = lambda k: np.asarray(inputs[k], np.float32)
    sh = {}
    sh["w_ada"] = np.ascontiguousarray(f("w_ada"))
    sh["b_ada"] = np.ascontiguousarray(_pcols(f("b_ada")).reshape(128, DEPTH * 48))
    gn = np.stack([f("g_norm1"), f("g_norm2"), f("g_mix")], axis=1)
    gn = _pcols(gn).reshape(128, DEPTH * 3 * 8)
    sh["gn"] = np.ascontiguousarray(np.concatenate([gn, _pcols(f("g_final"))], axis=1))
    sh["w_in"] = np.ascontiguousarray(f("w_in")[:, :, w_in_ext_cols()])
    cw = f("conv_w")
    cw = cw.reshape(DEPTH, 3, 2, 128).transpose(3, 0, 2, 1)
    sh["convw"] = np.ascontiguousarray(cw.reshape(128, DEPTH * 6))
    ridx, cidx, mask = na_table_indices()
    rpb = f("na_rpb")
    g = rpb[:, :, ridx, cidx]
    sh["rpbg"] = np.ascontiguousarray(g.transpose(0, 1, 3, 2, 4).reshape(DEPTH, 6, 128, NT * 128))
    sh["sink"] = np.ascontiguousarray(np.broadcast_to(f("sw_sink").reshape(1, DEPTH * 6), (128, DEPTH * 6)))
    sh["w_out"] = np.ascontiguousarray(f("w_out"))
    sh["ffn_wg"] = np.ascontiguousarray(f("ffn_w_gate")[0])
    sh["ffn_wu"] = np.ascontiguousarray(f("ffn_w_up")[0])
    sh["ffn_wd"] = np.ascontiguousarray(f("ffn_w_down")[0])
    sh["w_router"] = np.ascontiguousarray(_pcols(f("w_router")[0].T).transpose(0, 2, 1).reshape(128, 64))
    if SPARSE:
        gl = lambda w: np.ascontiguousarray(w.reshape(NE, 8, 128, 14, 256).transpose(0, 3, 2, 1, 4)).reshape(NE * 14 * 128, 2048)
        sh["moe_wg"] = gl(f("moe_w_gate")[0])
        sh["moe_wu"] = gl(f("moe_w_up")[0])
        sh["moe_wd"] = np.ascontiguousarray(f("moe_w_down")[0].reshape(NE, 4, 7, 128, D).transpose(0, 1, 3, 2, 4)).reshape(NE * 4 * 128, 7 * D)
    else:
        sh["moe_wg"] = np.ascontiguousarray(f("moe_w_gate")[0])
        sh["moe_wu"] = np.ascontiguousarray(f("moe_w_up")[0])
        sh["moe_wd"] = np.ascontiguousarray(f("moe_w_down")[0])
    kk = np.arange(128)
    ident = np.eye(128, dtype=np.float32)
    maskL = (kk[:, None] >= kk[None, :]).astype(np.float32)
    maskR = (kk[:, None] <= kk[None, :]).astype(np.float32)
    ustrict = (kk[:, None] < kk[None, :]).astype(np.float32)
    tokid = (np.arange(33)[None, :] * 128 + kk[:, None]).astype(np.float32)
    iotaGU = np.zeros((128, 112), np.float32)
    iotaGU[:, :14] = np.arange(14)[None, :] * 128 + kk[:, None]
    iotaD = np.zeros((128, 28), np.float32)
    iotaD[:, :4] = np.arange(4)[None, :] * 128 + kk[:, None]
    sh["cst"] = np.ascontiguousarray(np.concatenate([ident, maskL, maskR, mask.transpose(1, 0, 2).reshape(128, NT * 128), ustrict, tokid, iotaGU, iotaD], axis=1))
    C, S = rope_tables()
    sh["ropeC"] = C
    sh["ropeS"] = S
    return sh


def prep_core(inputs, b):
    x = np.asarray(inputs["x"][b], np.float32)
    ctx = np.asarray(inputs["ctx"][b], np.float32)
    xT = np.ascontiguousarray(np.concatenate([x.T, ctx.T], axis=1))
    c = _pcols(np.asarray(inputs["c"][b], np.float32))
    cc = _pcols(np.asarray(inputs["c_ctx"], np.float32))
    cv = np.ascontiguousarray(np.stack([c, cc], axis=2).reshape(128, 16))
    return {"xT": xT, "cv": cv}


_NC_CACHE = {}


def kernel(**inputs):
    B = inputs["x"].shape[0]
    if "nc" not in _NC_CACHE:
        _NC_CACHE["nc"] = build_nc()
    nc = _NC_CACHE["nc"]
    sh = prep_shared(inputs)
    in_maps = []
    for b in range(B):
        m = dict(sh)
        m.update(prep_core(inputs, b))
        in_maps.append(m)
    res = run_bass_kernel_spmd(nc, in_maps, core_ids=list(range(B)))
    out = np.stack([np.ascontiguousarray(r["yT"].T) for r in res.results], axis=0)
    return out.astype(np.float32)
```

```python
import numpy as np
from contextlib import ExitStack
import concourse.bass as bass
import concourse.mybir as mybir
from concourse.bass_utils import run_bass_kernel_spmd

F32 = mybir.dt.float32
BF16 = mybir.dt.bfloat16
AF = mybir.ActivationFunctionType
ALU = mybir.AluOpType

D = 1024
SEQ = 4096
CTX = 256
NTOK = SEQ + CTX
DEPTH = 2
D_IN_EXT = 3072
D_FF = 2816
D_FFE = 3584
NE = 8
EPS = 1e-6

ENGS = ["pe", "act", "dve", "pool", "sp"]
NRING = 12


class Op:
    __slots__ = ("fn", "waits", "tok", "dma", "kind", "snap")

    def __init__(self, fn, dma):
        self.fn = fn
        self.waits = []
        self.tok = None
        self.dma = dma
        self.kind = None
        self.snap = None


class Prog:
    def __init__(self, nc):
        self.nc = nc
        self.ops = {e: [] for e in ENGS}
        self.cnt = {e: 0 for e in ENGS}
        self.dcnt = {e: 0 for e in ENGS}
        self.ringval = {e: [0] * NRING for e in ENGS}
        self.kw = {}
        self.kr = {}
        self.waited = {e: {} for e in ENGS}
        self.needed = set()

    def _wait(self, op, eng, sem, val):
        w = self.waited[eng]
        if w.get(sem, 0) >= val:
            return
        w[sem] = val
        op.waits.append((sem, val))
        if isinstance(sem, str):
            self.needed.add((sem, val))

    def emit(self, eng, fn, reads=(), writes=(), dma=False):
        op = Op(fn, dma)
        mysem = eng
        if dma:
            k = self.dcnt[eng]
            self.dcnt[eng] += 1
            r = k % NRING
            ring = (eng, r)
            pv = self.ringval[eng][r]
            if pv > 0:
                self._wait(op, eng, ring, pv)
            self.ringval[eng][r] = pv + 16
            tok = (ring, pv + 16)
        else:
            self.cnt[eng] += 1
            tok = (eng, self.cnt[eng])
        op.tok = tok
        for key in reads:
            for sem, val in self.kw.get(key, {}).items():
                self._wait(op, eng, sem, val)
        skip_same = (not dma) and (eng == "pe" or not STRICT)
        for key in writes:
            for sem, val in self.kw.get(key, {}).items():
                if sem == mysem and skip_same:
                    continue
                self._wait(op, eng, sem, val)
            for sem, val in self.kr.get(key, {}).items():
                if sem == mysem and skip_same:
                    continue
                self._wait(op, eng, sem, val)
        for key in reads:
            self.kr.setdefault(key, {})[tok[0]] = tok[1]
        for key in writes:
            self.kw.setdefault(key, {})[tok[0]] = tok[1]
            self.kr[key] = {}
        self.ops[eng].append(op)
        return op

    def _all_tokens(self):
        toks = []
        for e in ENGS:
            if self.cnt[e] > 0:
                toks.append((e, self.cnt[e]))
            for r in range(NRING):
                if self.ringval[e][r] > 0:
                    toks.append(((e, r), self.ringval[e][r]))
        return toks

    def barrier(self):
        toks = self._all_tokens()
        for e in ENGS:
            op = Op(None, False)
            for sem, val in toks:
                self._wait(op, e, sem, val)
            self.ops[e].append(op)

    def site(self, sidx, reads=()):
        for e in ENGS:
            op = Op(None, False)
            op.kind = ("site", sidx)
            op.snap = (self.cnt[e], list(self.ringval[e]))
            for key in reads:
                for sem, val in self.kw.get(key, {}).items():
                    self._wait(op, e, sem, val)
            self.ops[e].append(op)

    def end_sites(self):
        for e in ENGS:
            op = Op(None, False)
            op.kind = ("end",)
            op.snap = (self.cnt[e], list(self.ringval[e]))
            self.ops[e].append(op)
        self.waited = {e: {} for e in ENGS}

    def finish(self):
        op = Op(None, False)
        for sem, val in self._all_tokens():
            self._wait(op, "sp", sem, val)
        self.ops["sp"].append(op)

    def replay(self, stack):
        nc = self.nc
        rank = {}
        for e in ENGS:
            vals = sorted(v for (s, v) in self.needed if s == e)
            rank[e] = {v: i + 1 for i, v in enumerate(vals)}
        sems = {}
        for e in ENGS:
            sems[e] = stack.enter_context(nc.semaphore("s_" + e))
            for r in range(NRING):
                if self.ringval[e][r] > 0:
                    sems[(e, r)] = stack.enter_context(nc.semaphore("r_%s_%d" % (e, r)))
        block = stack.enter_context(nc.Block())

        import bisect
        nsorted = {e: sorted(rank[e].keys()) for e in ENGS}

        def run(e, eng):
            rk = rank[e]
            end_snap = None
            for op in self.ops[e]:
                if op.kind is not None and op.kind[0] == "end":
                    end_snap = op.snap
            ctx_stack = []
            loaded = False
            rank_of = lambda c: bisect.bisect_right(nsorted[e], c)
            for op in self.ops[e]:
                for sem, val in op.waits:
                    if isinstance(sem, str):
                        val = rank[sem][val]
                    eng.wait_ge(sems[sem], val)
                if op.kind is not None:
                    if op.kind[0] == "site":
                        if not loaded:
                            eng.reg_load(self.pred_regs[e], self.pred_ap)
                            loaded = True
                        cnt_s, ring_s = op.snap
                        cnt_e, ring_e = end_snap
                        cm = eng.If_lt(self.pred_regs[e], op.kind[1] + 1)
                        cm.__enter__()
                        r_s, r_e = rank_of(cnt_s), rank_of(cnt_e)
                        if r_s > 0:
                            eng.wait_ge(sems[e], r_s)
                        for r in range(NRING):
                            if ring_s[r] > 0:
                                eng.wait_ge(sems[(e, r)], ring_s[r])
                        if r_e > r_s:
                            eng.sem_inc(sems[e], r_e - r_s)
                        for r in range(NRING):
                            if ring_e[r] > ring_s[r]:
                                eng.sem_inc(sems[(e, r)], ring_e[r] - ring_s[r])
                        cm.__exit__(None, None, None)
                        cm2 = eng.Else()
                        cm2.__enter__()
                        ctx_stack.append(cm2)
                    else:
                        while ctx_stack:
                            ctx_stack.pop().__exit__(None, None, None)
                    continue
                if op.fn is None:
                    continue
                ins = op.fn(eng)
                if op.dma:
                    ins.then_inc(sems[op.tok[0]], 16)
                elif op.tok[1] in rk:
                    ins.then_inc(sems[e], 1)

        @block.tensor
        def _(eng):
            run("pe", eng)

        @block.scalar
        def _(eng):
            run("act", eng)

        @block.vector
        def _(eng):
            run("dve", eng)

        @block.gpsimd
        def _(eng):
            run("pool", eng)

        @block.sync
        def _(eng):
            run("sp", eng)


def _rs(r):
    return min(max(r - 4, 0), 56)


def na_plan():
    tables = []
    for dl in (-4, -2, 0, 2, 4):
        r0 = 20
        pat = tuple(tuple(1 if _rs(r0 + qr) <= r0 + dl + kr < _rs(r0 + qr) + 8 else 0 for qr in (0, 1)) for kr in (0, 1))
        tables.append((dl, pat))
    tiles = []
    for i in range(32):
        r0 = 2 * i
        lo = min(_rs(r0), _rs(r0 + 1))
        hi = max(_rs(r0), _rs(r0 + 1)) + 7
        lst = []
        for k0 in range((lo // 2) * 2, hi + 1, 2):
            pat = tuple(tuple(1 if _rs(r0 + qr) <= k0 + kr < _rs(r0 + qr) + 8 else 0 for qr in (0, 1)) for kr in (0, 1))
            if not any(any(p) for p in pat):
                continue
            key = (k0 - r0, pat)
            if key not in tables:
                tables.append(key)
            lst.append((k0, tables.index(key)))
        tiles.append(lst)
    return tiles, tables


NA_TILES, NA_TABLES = na_plan()
NT = len(NA_TABLES)


def na_table_indices():
    ridx = np.zeros((NT, 128, 128), np.int64)
    cidx = np.zeros((NT, 128, 128), np.int64)
    mask = np.zeros((NT, 128, 128), np.float32)
    kk = np.arange(128)
    kr, kc = kk // 64, kk % 64
    qr, qc = kk // 64, kk % 64
    cstart = np.clip(qc - 8, 0, 48)
    for t, (dl, pat) in enumerate(NA_TABLES):
        pat = np.array(pat)
        drow = dl + kr[:, None] - qr[None, :]
        rv = pat[kr[:, None], qr[None, :]]
        cv = (kc[:, None] >= cstart[None, :]) & (kc[:, None] < cstart[None, :] + 16)
        m = (rv == 1) & cv
        ridx[t] = np.where(m, np.clip(drow + 7, 0, 14), 0)
        cidx[t] = np.where(m, np.clip(kc[:, None] - qc[None, :] + 15, 0, 30), 0)
        mask[t] = m.astype(np.float32)
    return ridx, cidx, mask


def w_in_ext_cols():
    cols = list(range(0, 768))
    cols += list(range(768, 1152))
    swq0 = 1152
    perm_heads = [0, 3, 1, 4, 2, 5]
    swq = []
    swqs = []
    for h in perm_heads:
        base = swq0 + h * 64
        swq += list(range(base, base + 64))
        swqs += [base + (d + 16 if (d % 32) < 16 else d - 16) for d in range(64)]
    cols += swq + swqs
    cols += list(range(1536, 1920))
    swk0 = 2304
    swk = list(range(swk0, swk0 + 128))
    swks = []
    for h in range(2):
        base = swk0 + h * 64
        swks += [base + (d + 16 if (d % 32) < 16 else d - 16) for d in range(64)]
    cols += swk + swks
    cols += list(range(1920, 2304))
    cols += list(range(2432, 2560))
    assert len(cols) == D_IN_EXT
    return np.array(cols)


def rope_tables():
    t = np.arange(SEQ)
    row = (t // 64).astype(np.float32)
    col = (t % 64).astype(np.float32)
    inv = (10000.0 ** (-np.arange(16, dtype=np.float32) / 16)).astype(np.float32)
    C = np.zeros((64, SEQ), np.float32)
    S = np.zeros((64, SEQ), np.float32)
    for a, pos in enumerate((row, col)):
        ang = (pos[None, :] * inv[:, None]).astype(np.float32)
        c, s = np.cos(ang), np.sin(ang)
        C[a * 32:a * 32 + 16] = c
        C[a * 32 + 16:a * 32 + 32] = c
        S[a * 32:a * 32 + 16] = -s
        S[a * 32 + 16:a * 32 + 32] = s
    return np.concatenate([C, C], 0), np.concatenate([S, S], 0)


SB_LO = 16640
SB_HI = 229376


class Arena:
    def __init__(self, nc):
        self.nc = nc
        self.off = SB_LO
        self.n = 0

    def alloc(self, name, shape, dt):
        esz = 2 if dt == BF16 else 4
        nbytes = esz
        for s in shape[1:]:
            nbytes *= s
        nbytes = (nbytes + 63) // 64 * 64
        assert self.off + nbytes <= SB_HI, "SBUF overflow %s %d" % (name, self.off + nbytes - SB_HI)
        self.n += 1
        t = self.nc.alloc_sbuf_tensor_at("%s_%d" % (name, self.n), list(shape), dt, offset=self.off)
        self.off += nbytes
        return t

    def mark(self):
        return self.off

    def reset(self, m):
        self.off = m


SPARSE = True
SKIP = True
MC_CFG = 1152
STRICT = True


def build_nc(dbg=False, nlayers=DEPTH, stop_phase=99):
    nc = bass.Bass("TRN2", target_bir_lowering=False)
    I = "ExternalInput"
    xT_in = nc.dram_tensor("xT", [D, NTOK], F32, kind=I).ap()
    cv_in = nc.dram_tensor("cv", [128, 16], F32, kind=I).ap()
    wada_in = nc.dram_tensor("w_ada", [DEPTH, D, 6 * D], F32, kind=I).ap()
    bada_in = nc.dram_tensor("b_ada", [128, DEPTH * 48], F32, kind=I).ap()
    gn_in = nc.dram_tensor("gn", [128, DEPTH * 3 * 8 + 8], F32, kind=I).ap()
    win_in = nc.dram_tensor("w_in", [DEPTH, D, D_IN_EXT], F32, kind=I).ap()
    convw_in = nc.dram_tensor("convw", [128, DEPTH * 2 * 3], F32, kind=I).ap()
    rpbg_in = nc.dram_tensor("rpbg", [DEPTH, 6, 128, NT * 128], F32, kind=I).ap()
    sink_in = nc.dram_tensor("sink", [128, DEPTH * 6], F32, kind=I).ap()
    wout_in = nc.dram_tensor("w_out", [DEPTH, D, D], F32, kind=I).ap()
    fwg_in = nc.dram_tensor("ffn_wg", [D, D_FF], F32, kind=I).ap()
    fwu_in = nc.dram_tensor("ffn_wu", [D, D_FF], F32, kind=I).ap()
    fwd_in = nc.dram_tensor("ffn_wd", [D_FF, D], F32, kind=I).ap()
    wr_in = nc.dram_tensor("w_router", [128, 64], F32, kind=I).ap()
    if SPARSE:
        mwg_in = nc.dram_tensor("moe_wg", [NE * 14 * 128, 2048], F32, kind=I).ap()
        mwu_in = nc.dram_tensor("moe_wu", [NE * 14 * 128, 2048], F32, kind=I).ap()
        mwd_in = nc.dram_tensor("moe_wd", [NE * 4 * 128, 7 * D], F32, kind=I).ap()
    else:
        mwg_in = nc.dram_tensor("moe_wg", [NE, D, D_FFE], F32, kind=I).ap()
        mwu_in = nc.dram_tensor("moe_wu", [NE, D, D_FFE], F32, kind=I).ap()
        mwd_in = nc.dram_tensor("moe_wd", [NE, D_FFE, D], F32, kind=I).ap()
    CSTW = 128 * 3 + NT * 128 + 128 + 33 + 112 + 28
    cst_in = nc.dram_tensor("cst", [128, CSTW], F32, kind=I).ap()
    ropeC_in = nc.dram_tensor("ropeC", [128, SEQ], F32, kind=I).ap()
    ropeS_in = nc.dram_tensor("ropeS", [128, SEQ], F32, kind=I).ap()
    yT_out = nc.dram_tensor("yT", [D, SEQ], F32, kind="ExternalOutput").ap()
    skind = "ExternalOutput" if dbg else "Internal"
    xs = nc.dram_tensor("xs", [D, NTOK], F32, kind=skind).ap()
    qd = nc.dram_tensor("qd", [10, 128, NTOK], BF16, kind=skind).ap()
    dbgo = nc.dram_tensor("dbgo", [128, 4096], F32, kind=skind).ap() if dbg else None
    MC = MC_CFG
    MKS = MC // 128
    NSLOT = (2 * SEQ) // MC + NE
    MINU = -(-2 * SEQ // MC)
    I32 = mybir.dt.int32
    h2d = nc.dram_tensor("h2d", [SEQ + 128, D], BF16, kind="Internal").ap()
    macc = nc.dram_tensor("macc", [SEQ + 128, D], F32, kind=skind).ap()
    L2 = nc.dram_tensor("L2", [NSLOT * MC, 2], I32, kind=skind).ap()

    xT3 = xT_in.rearrange("(c p) t -> p c t", p=128)
    xs3 = xs.rearrange("(c p) t -> p c t", p=128)
    yT3 = yT_out.rearrange("(c p) t -> p c t", p=128)
    qd3 = qd.rearrange("k p t -> p k t")

    P = Prog(nc)
    A = Arena(nc)
    em = P.emit

    st = ExitStack()
    with st:
        psA = st.enter_context(nc.psum_tensor("psA", [128, 1024], F32))
        psB = st.enter_context(nc.psum_tensor("psB", [128, 1024], F32))
        ps4 = st.enter_context(nc.psum_tensor("ps4", [128, 512], F32))
        ps5 = st.enter_context(nc.psum_tensor("ps5", [128, 512], F32))
        ps6 = st.enter_context(nc.psum_tensor("ps6", [128, 1024], BF16))
        ps7 = st.enter_context(nc.psum_tensor("ps7", [128, 512], F32))
        PSH = [(psA, 0, "psA0"), (psA, 512, "psA1"), (psB, 0, "psB0"), (psB, 512, "psB1")]

        ident_f = A.alloc("ident_f", [128, 128], F32)
        ident_b = A.alloc("ident_b", [128, 128], BF16)
        ones_f = A.alloc("ones_f", [128, 128], F32)
        onesD = A.alloc("onesD", [128, 128], BF16)
        ones256 = A.alloc("ones256", [128, 128], BF16)
        maskLR = A.alloc("maskLR", [128, 256], BF16)
        maskNA = A.alloc("maskNA", [128, NT * 128], BF16)
        cact = A.alloc("cact", [128, 16], F32)
        adav = A.alloc("adav", [128, DEPTH, 48, 2], F32)
        Amod = A.alloc("Amod", [128, DEPTH, 2, 2, 8], F32)
        gn = A.alloc("gn", [128, DEPTH * 3 * 8 + 8], F32)
        bada = A.alloc("bada", [128, DEPTH * 48], F32)
        convw = A.alloc("convw", [128, DEPTH * 6], F32)
        esink = A.alloc("esink", [128, DEPTH * 6], F32)
        wr_sb = A.alloc("wr_sb", [128, 64], F32)
        ustrict = A.alloc("ustrict", [128, 128], F32)
        tokid = A.alloc("tokid", [128, 33], mybir.dt.int32)
        iotaGU = A.alloc("iotaGU", [128, 112], F32)
        iotaD = A.alloc("iotaD", [128, 28], F32)
        base_mark = A.mark()

        cstage = A.alloc("cstage", [128, CSTW], F32)
        em("sp", lambda e: e.dma_start(out=cstage[:], in_=cst_in), writes=["cstage"], dma=True)
        em("sp", lambda e: e.dma_start(out=cact[:], in_=cv_in), writes=["cact"], dma=True)
        em("sp", lambda e: e.dma_start(out=gn[:], in_=gn_in), writes=["gn"], dma=True)
        em("sp", lambda e: e.dma_start(out=bada[:], in_=bada_in), writes=["bada"], dma=True)
        em("sp", lambda e: e.dma_start(out=convw[:], in_=convw_in), writes=["convw"], dma=True)
        em("sp", lambda e: e.dma_start(out=esink[:], in_=sink_in), writes=["esink"], dma=True)
        em("sp", lambda e: e.dma_start(out=wr_sb[:], in_=wr_in), writes=["wr_sb"], dma=True)
        em("dve", lambda e: e.tensor_copy(out=ident_f[:], in_=cstage[:, 0:128]), reads=["cstage"], writes=["ident_f"])
        em("dve", lambda e: e.tensor_copy(out=ident_b[:], in_=cstage[:, 0:128]), reads=["cstage"], writes=["ident_b"])
        em("dve", lambda e: e.tensor_copy(out=maskLR[:], in_=cstage[:, 128:384]), reads=["cstage"], writes=["maskLR"])
        em("dve", lambda e: e.tensor_copy(out=maskNA[:], in_=cstage[:, 384:384 + NT * 128]), reads=["cstage"], writes=["maskNA"])
        em("dve", lambda e: e.tensor_copy(out=ustrict[:], in_=cstage[:, 384 + NT * 128:384 + NT * 128 + 128]), reads=["cstage"], writes=["ustrict"])
        em("dve", lambda e: e.tensor_copy(out=tokid[:], in_=cstage[:, 384 + NT * 128 + 128:384 + NT * 128 + 161]), reads=["cstage"], writes=["tokid"])
        em("dve", lambda e: e.tensor_copy(out=iotaGU[:], in_=cstage[:, 384 + NT * 128 + 161:384 + NT * 128 + 273]), reads=["cstage"], writes=["iotaGU"])
        em("dve", lambda e: e.tensor_copy(out=iotaD[:], in_=cstage[:, 384 + NT * 128 + 273:384 + NT * 128 + 301]), reads=["cstage"], writes=["iotaD"])
        em("pool", lambda e: e.memset(ones_f[:], 1.0), writes=["ones_f"])
        em("pool", lambda e: e.memset(onesD[:], 1.0 / D), writes=["onesD"])
        em("pool", lambda e: e.memset(ones256[:], 1.0 / 256), writes=["ones256"])
        em("act", lambda e: e.activation(out=esink[:], in_=esink[:], func=AF.Exp), reads=["esink"], writes=["esink"])
        em("act", lambda e: e.activation(out=cact[:], in_=cact[:], func=AF.Silu), reads=["cact"], writes=["cact"])

        wa = [A.alloc("wa%d" % i, [128, 8, 512], F32) for i in range(2)]
        cact3 = cact[:].rearrange("p (c w) -> p c w", w=2)
        for l in range(nlayers if stop_phase >= 0 else 0):
            wsrc = wada_in[l].rearrange("(c p) n -> p c n", p=128)
            for pc in range(12):
                s = pc % 2
                em("sp", lambda e, s=s, pc=pc, wsrc=wsrc: e.dma_start(out=wa[s][:], in_=wsrc[:, :, pc * 512:(pc + 1) * 512]),
                   writes=["wa%d" % s], dma=True)
                import os
                P0 = int(os.environ.get("P0", "9"))
                for nn in range(4 if P0 >= 1 else 0):
                    ch = pc * 4 + nn
                    for c in range(8):
                        em("pe", lambda e, s=s, nn=nn, c=c, ch=ch: e.matmul(
                            ps7[:, ch * 2:ch * 2 + 2], lhsT=wa[s][:, c, nn * 128:(nn + 1) * 128], rhs=cact3[:, c, :],
                            start=(c == 0), stop=(c == 7)), reads=["wa%d" % s, "cact"], writes=["ps7"])
            ps7v = ps7[:, 0:96].rearrange("p (k w) -> p k w", w=2)
            for w in range(2 if P0 >= 2 else 0):
                em("dve", lambda e, l=l, w=w, ps7v=ps7v: e.tensor_tensor(
                    out=adav[:, l, :, w], in0=ps7v[:, :, w], in1=bada[:, l * 48:(l + 1) * 48], op=ALU.add),
                    reads=["ps7", "bada"], writes=["adav"])
                for k in range(2 if P0 >= 3 else 0):
                    sc0 = 8 + 24 * k
                    em("dve", lambda e, l=l, w=w, k=k, sc0=sc0: e.scalar_tensor_tensor(
                        out=Amod[:, l, k, w, :], in0=adav[:, l, sc0:sc0 + 8, w], scalar=1.0,
                        in1=gn[:, (l * 3 + k) * 8:(l * 3 + k) * 8 + 8], op0=ALU.add, op1=ALU.mult),
                        reads=["adav", "gn"], writes=["Amod"])
        if dbg:
            em("sp", lambda e: e.dma_start(out=dbgo[:, 0:DEPTH * 96], in_=adav[:].rearrange("p l k w -> p (l k w)")), reads=["adav"], writes=["dbgo"], dma=True)
            em("sp", lambda e: e.dma_start(out=dbgo[:, 256:256 + DEPTH * 32], in_=Amod[:].rearrange("p l k w c -> p (l k w c)")), reads=["Amod"], writes=["dbgo"], dma=True)
        P.barrier()
        A.reset(base_mark)
        import os
        P1 = int(os.environ.get("P1", "9"))

        def modcols(l, k, w):
            a = lambda c: Amod[:, l, k, w, c:c + 1]
            b = lambda c: adav[:, l, 24 * k + c, w:w + 1]
            g = lambda c: adav[:, l, 24 * k + 16 + c, w:w + 1]
            return a, b, g

        def norm_mod(xt, xkey, TT, a, b, sq, sqkey, rstd, rkey, tmp2, tkeys, outs, okey, psbank, pskey):
            em("act", lambda e: e.activation(out=sq[:, :, 0:TT], in_=xt[:, :, 0:TT], func=AF.Square), reads=[xkey], writes=[sqkey])
            for c in range(8):
                em("pe", lambda e, c=c: e.matmul(psbank[:, 0:TT], lhsT=onesD[:], rhs=sq[:, c, 0:TT], start=(c == 0), stop=(c == 7)),
                   reads=[sqkey, "onesD"], writes=[pskey])
            em("act", lambda e: e.activation(out=rstd[:, 0:TT], in_=psbank[:, 0:TT], func=AF.Sqrt, bias=EPS, scale=1.0), reads=[pskey], writes=[rkey])
            em("dve", lambda e: e.reciprocal(out=rstd[:, 0:TT], in_=rstd[:, 0:TT]), reads=[rkey], writes=[rkey])
            for c in range(8):
                t = tmp2[c % 2]
                tk = tkeys[c % 2]
                em("dve", lambda e, c=c, t=t: e.scalar_tensor_tensor(out=t[:, 0:TT], in0=xt[:, c, 0:TT], scalar=a(c), in1=rstd[:, 0:TT],
                                                               op0=ALU.mult, op1=ALU.mult), reads=[xkey, rkey, "Amod"], writes=[tk])
                em("act", lambda e, c=c, t=t: e.activation(out=outs(c), in_=t[:, 0:TT], func=AF.Identity, bias=b(c), scale=1.0),
                   reads=[tk, "adav"], writes=[okey])


        def moe_sparse(l):
            a2, b2, g2 = modcols(l, 1, 0)
            NJ = D_FFE // 128
            mk0 = A.mark()
            Ui = A.alloc("Ui", [128, 1], I32)
            idxGU = A.alloc("idxGU", [128, NSLOT * 14], I32)
            idxD = A.alloc("idxD", [128, NSLOT * 4], I32)
            mk1_ = A.mark()
            idxtf = A.alloc("idxtf", [128, 112], F32)
            sel1 = A.alloc("sel1", [128, 32, 8], F32)
            sel2 = A.alloc("sel2", [128, 32, 8], F32)
            rank = A.alloc("rank", [128, 32, 8], F32)
            gate = A.alloc("gate", [128, 32, 2], F32)
            basec = A.alloc("basec", [128, 8], F32)
            rt = A.alloc("rt", [128, 96], F32)
            slots = A.alloc("slots", [128, 8], F32)
            cum = A.alloc("cum", [128, 8], F32)
            sbC = A.alloc("sbC", [128, 8], F32)
            posv = A.alloc("posv", [128, 32, 8], F32)
            posf = A.alloc("posf", [128, 32, 2], F32)
            posi2 = A.alloc("posi", [128, 64], I32)
            posi = posi2[:].rearrange("p (g k) -> p g k", k=2)
            entall2 = A.alloc("entall", [128, 128], I32)
            entall = entall2[:].rearrange("p (g k w) -> p g k w", k=2, w=2)
            eidf = A.alloc("eidf", [128, NSLOT], F32)
            eidi = A.alloc("eidi", [128, NSLOT], I32)
            fill = A.alloc("fill", [128, NSLOT * MKS, 2], I32)
            x3 = A.alloc("mx3", [128, 8, 512], F32)
            h32 = A.alloc("mh32", [128, 8, 512], F32)
            sq3 = A.alloc("msq3", [128, 8, 512], BF16)
            rstd3 = A.alloc("mrstd3", [128, 512], F32)
            tmp3 = [A.alloc("mtmp3", [128, 512], F32) for _ in range(2)]
            hTb = A.alloc("mhTb", [128, 8, 512], BF16)
            htok = [A.alloc("mhtok", [128, D], BF16) for _ in range(2)]
            zt = A.alloc("mzt", [128, D], F32)
            selt = A.alloc("mselt", [128, 8], F32)
            em("pool", lambda e: e.memset(zt[:], 0.0), writes=["mzt"])
            em("pool", lambda e: e.memset(basec[:], 0.0), writes=["basec"])
            em("pool", lambda e: e.memset(htok[0][:], 0.0), writes=["mhtok0"])
            em("sp", lambda e: e.dma_start(out=h2d[SEQ:SEQ + 128, :], in_=htok[0][:]), reads=["mhtok0"], writes=["h2d"], dma=True)
            for k in range(32):
                em("sp", lambda e, k=k: e.dma_start(out=macc[k * 128:(k + 1) * 128, :], in_=zt[:]), reads=["mzt"], writes=["maccz%d" % k], dma=True)
            em("sp", lambda e: e.dma_start(out=macc[SEQ:SEQ + 128, :], in_=zt[:]), reads=["mzt"], writes=["maccz32"], dma=True)
            em("dve", lambda e: e.tensor_copy(out=fill[:, :, 0], in_=tokid[:, 32:33].to_broadcast([128, NSLOT * MKS])), reads=["tokid"], writes=["fill"])
            em("pool", lambda e: e.memset(fill[:, :, 1:2], 0), writes=["fill"])
            em("sp", lambda e: e.dma_start(out=L2.rearrange("(k p) w -> p k w", p=128), in_=fill[:]), reads=["fill"], writes=["L2"], dma=True)
            h32_2 = [h32, A.alloc("mh32b", [128, 8, 512], F32)]
            hTb_2 = [hTb, A.alloc("mhTbb", [128, 8, 512], BF16)]

            def norm_part(ti):
                pb = ti % 2
                t0 = ti * 512
                h32_, hTb_ = h32_2[pb], hTb_2[pb]
                em("sp", lambda e: e.dma_start(out=x3[:], in_=xs3[:, :, t0:t0 + 512]), reads=["xs%d" % ti], writes=["mx3"], dma=True)
                norm_mod(x3, "mx3", 512, a2, b2, sq3, "msq3", rstd3, "mrstd3", tmp3, ["mtmp30", "mtmp31"],
                         lambda c: h32_[:, c, :], "mh32%d" % pb, ps4, "ps4")
                em("pool", lambda e: e.tensor_copy(out=hTb_[:], in_=h32_[:]), reads=["mh32%d" % pb], writes=["mhTb%d" % pb])

            norm_part(0)
            for ti in range(8):
                t0 = ti * 512
                if ti + 1 < 8:
                    norm_part(ti + 1)
                h32 = h32_2[ti % 2]
                hTb = hTb_2[ti % 2]
                hkey = "mh32%d" % (ti % 2)
                bkey = "mhTb%d" % (ti % 2)
                for sub in range(4):
                    gsub = ti * 4 + sub
                    hs = gsub % 2
                    for c in range(8):
                        em("pe", lambda e, c=c, sub=sub, hTb=hTb: e.transpose(out=ps6[:, c * 128:(c + 1) * 128], in_=hTb[:, c, sub * 128:(sub + 1) * 128], identity=ident_b[:]),
                           reads=[bkey, "ident_b"], writes=["ps6"])
                    em("act", lambda e, hs=hs: e.activation(out=htok[hs][:], in_=ps6[:], func=AF.Copy), reads=["ps6"], writes=["mhtok%d" % hs])
                    em("sp", lambda e, hs=hs, gsub=gsub: e.dma_start(out=h2d[gsub * 128:(gsub + 1) * 128, :], in_=htok[hs][:]),
                       reads=["mhtok%d" % hs], writes=["h2d%d" % gsub], dma=True)
                    for c in range(8):
                        em("pe", lambda e, c=c, sub=sub, h32=h32: e.matmul(ps7[:, 0:8], lhsT=h32[:, c, sub * 128:(sub + 1) * 128], rhs=wr_sb[:, c * 8:(c + 1) * 8],
                                                                start=(c == 0), stop=(c == 7)), reads=[hkey, "wr_sb"], writes=["ps7"])
                    lg = rt[:, 0:8]
                    m8 = rt[:, 8:16]
                    dd = rt[:, 32:33]
                    ga = gate[:, gsub, 0:1]
                    gb = gate[:, gsub, 1:2]
                    mk1 = sel1[:, gsub, :]
                    mk2 = sel2[:, gsub, :]
                    em("dve", lambda e, lg=lg: e.tensor_copy(out=lg, in_=ps7[:, 0:8]), reads=["ps7"], writes=["rt"])
                    em("dve", lambda e, lg=lg, m8=m8: e.max(out=m8, in_=lg), reads=["rt"], writes=["rt"])
                    em("dve", lambda e, lg=lg, m8=m8, mk1=mk1: e.tensor_scalar(out=mk1, in0=lg, scalar1=m8[:, 0:1], scalar2=None, op0=ALU.is_equal), reads=["rt"], writes=["sel"])
                    em("dve", lambda e, lg=lg, m8=m8, mk2=mk2: e.tensor_scalar(out=mk2, in0=lg, scalar1=m8[:, 1:2], scalar2=None, op0=ALU.is_equal), reads=["rt"], writes=["sel"])
                    em("dve", lambda e, m8=m8, dd=dd: e.tensor_tensor(out=dd, in0=m8[:, 1:2], in1=m8[:, 0:1], op=ALU.subtract), reads=["rt"], writes=["rt"])
                    em("act", lambda e, dd=dd: e.activation(out=dd, in_=dd, func=AF.Exp), reads=["rt"], writes=["rt"])
                    em("dve", lambda e, dd=dd, ga=ga: e.tensor_scalar(out=ga, in0=dd, scalar1=1.0, scalar2=None, op0=ALU.add), reads=["rt"], writes=["gate"])
                    em("dve", lambda e, ga=ga: e.reciprocal(out=ga, in_=ga), reads=["gate"], writes=["gate"])
                    em("dve", lambda e, dd=dd, ga=ga, gb=gb: e.tensor_tensor(out=gb, in0=dd, in1=ga, op=ALU.mult), reads=["rt", "gate"], writes=["gate"])
                    em("dve", lambda e, mk1=mk1, mk2=mk2: e.tensor_tensor(out=selt[:], in0=mk1, in1=mk2, op=ALU.add), reads=["sel"], writes=["mselt"])
                    em("pe", lambda e: e.matmul(ps7[:, 8:16], lhsT=ustrict[:], rhs=selt[:], start=True, stop=True), reads=["mselt", "ustrict"], writes=["ps7"])
                    em("pe", lambda e: e.matmul(ps7[:, 16:24], lhsT=ones_f[:], rhs=selt[:], start=True, stop=True), reads=["mselt", "ones_f"], writes=["ps7"])
                    em("dve", lambda e, gsub=gsub: e.tensor_tensor(out=rank[:, gsub, :], in0=ps7[:, 8:16], in1=basec[:], op=ALU.add), reads=["ps7", "basec"], writes=["rank"])
                    em("dve", lambda e: e.tensor_tensor(out=basec[:], in0=ps7[:, 16:24], in1=basec[:], op=ALU.add), reads=["ps7", "basec"], writes=["basec"])
            em("dve", lambda e: e.tensor_scalar(out=slots[:], in0=basec[:], scalar1=0.5, scalar2=None, op0=ALU.is_gt), reads=["basec"], writes=["slots"])
            for k in range(1, -(-SEQ // MC)):
                em("dve", lambda e, k=k: e.tensor_scalar(out=cum[:], in0=basec[:], scalar1=float(k * MC) + 0.5, scalar2=None, op0=ALU.is_gt), reads=["basec"], writes=["cum"])
                em("dve", lambda e: e.tensor_tensor(out=slots[:], in0=slots[:], in1=cum[:], op=ALU.add), reads=["slots", "cum"], writes=["slots"])
            em("dve", lambda e: e.tensor_copy(out=cum[:, 0:1], in_=slots[:, 0:1]), reads=["slots"], writes=["cum"])
            for ex in range(1, 8):
                em("dve", lambda e, ex=ex: e.tensor_tensor(out=cum[:, ex:ex + 1], in0=cum[:, ex - 1:ex], in1=slots[:, ex:ex + 1], op=ALU.add), reads=["cum", "slots"], writes=["cum"])
            em("dve", lambda e: e.tensor_tensor(out=sbC[:], in0=cum[:], in1=slots[:], op=ALU.subtract), reads=["cum", "slots"], writes=["sbC"])
            em("dve", lambda e: e.tensor_scalar(out=sbC[:], in0=sbC[:], scalar1=float(MC), scalar2=None, op0=ALU.mult), reads=["sbC"], writes=["sbC"])
            em("dve", lambda e: e.tensor_tensor(out=posv[:], in0=rank[:], in1=sbC[:].unsqueeze(1).to_broadcast([128, 32, 8]), op=ALU.add), reads=["rank", "sbC"], writes=["posv"])
            for k, sel in enumerate((sel1, sel2)):
                em("dve", lambda e, sel=sel: e.tensor_tensor(out=rank[:], in0=posv[:], in1=sel[:], op=ALU.mult), reads=["posv", "sel", "rank"], writes=["rank"])
                em("dve", lambda e, k=k: e.reduce_sum(out=posf[:, :, k], in_=rank[:], axis=mybir.AxisListType.X), reads=["rank"], writes=["posf"])
            em("dve", lambda e: e.tensor_copy(out=posi, in_=posf[:]), reads=["posf"], writes=["posi"])
            for k in range(2):
                em("dve", lambda e, k=k: e.tensor_copy(out=entall[:, :, k, 0], in_=tokid[:, 0:32]), reads=["tokid"], writes=["entall"])
                em("dve", lambda e, k=k: e.tensor_copy(out=entall[:, :, k, 1], in_=gate[:, :, k].bitcast(I32)), reads=["gate"], writes=["entall"])
            for sl_ in range(NSLOT):
                em("dve", lambda e, sl_=sl_: e.tensor_scalar(out=rt[:, 40:48], in0=cum[:], scalar1=float(sl_) + 0.5, scalar2=None, op0=ALU.is_lt), reads=["cum"], writes=["rt"])
                em("dve", lambda e, sl_=sl_: e.reduce_sum(out=eidf[:, sl_:sl_ + 1], in_=rt[:, 40:48], axis=mybir.AxisListType.X), reads=["rt"], writes=["eidf"])
            em("dve", lambda e: e.tensor_scalar(out=eidf[:], in0=eidf[:], scalar1=7.0, scalar2=None, op0=ALU.min), reads=["eidf"], writes=["eidf"])
            em("dve", lambda e: e.tensor_copy(out=eidi[:], in_=eidf[:]), reads=["eidf"], writes=["eidi"])
            em("dve", lambda e: e.tensor_copy(out=Ui[:], in_=cum[:, 7:8]), reads=["cum"], writes=["Ui"])
            em("dve", lambda e: e.tensor_scalar(out=rt[:, 48:48 + NSLOT], in0=eidf[:], scalar1=float(14 * 128), scalar2=None, op0=ALU.mult), reads=["eidf"], writes=["rt"])
            for sl_ in range(NSLOT):
                em("dve", lambda e, sl_=sl_: e.tensor_scalar(out=idxtf[:, 0:14], in0=iotaGU[:, 0:14], scalar1=rt[:, 48 + sl_:49 + sl_], scalar2=None, op0=ALU.add), reads=["rt", "iotaGU"], writes=["idxtf"])
                em("dve", lambda e, sl_=sl_: e.tensor_copy(out=idxGU[:, sl_ * 14:(sl_ + 1) * 14], in_=idxtf[:, 0:14]), reads=["idxtf"], writes=["idxGU"])
            em("dve", lambda e: e.tensor_scalar(out=rt[:, 48:48 + NSLOT], in0=eidf[:], scalar1=float(4 * 128), scalar2=None, op0=ALU.mult), reads=["eidf", "idxtf"], writes=["rt"])
            for sl_ in range(NSLOT):
                em("dve", lambda e, sl_=sl_: e.tensor_scalar(out=idxtf[:, 0:4], in0=iotaD[:, 0:4], scalar1=rt[:, 48 + sl_:49 + sl_], scalar2=None, op0=ALU.add), reads=["rt", "iotaD"], writes=["idxtf"])
                em("dve", lambda e, sl_=sl_: e.tensor_copy(out=idxD[:, sl_ * 4:(sl_ + 1) * 4], in_=idxtf[:, 0:4]), reads=["idxtf"], writes=["idxD"])
            for gsub in range(32):
                for k in range(2):
                    em("pool", lambda e, gsub=gsub, k=k: e.indirect_dma_start(
                        out=L2[:, :], out_offset=bass.IndirectOffsetOnAxis(ap=posi2[:, gsub * 2 + k:gsub * 2 + k + 1], axis=0), in_=entall2[:, (gsub * 2 + k) * 2:(gsub * 2 + k) * 2 + 2], in_offset=None,
                        ), reads=["posi", "entall", "L2"], writes=["L2s%d" % (gsub * 2 + k)], dma=True)
            if dbg:
                em("sp", lambda e: e.dma_start(out=dbgo[:, 1024:1032], in_=basec[:]), reads=["basec"], writes=["dbgo"], dma=True)
                em("sp", lambda e: e.dma_start(out=dbgo[:, 1032:1040], in_=cum[:]), reads=["cum"], writes=["dbgo"], dma=True)
                em("sp", lambda e: e.dma_start(out=dbgo[:, 1040:1040 + NSLOT], in_=eidf[:]), reads=["eidf"], writes=["dbgo"], dma=True)
            P.barrier()
            A.reset(mk1_)
            hTg = [A.alloc("hTg", [128, 8, MC], BF16) for _ in range(2)]
            inter = A.alloc("minter", [128, NJ, MC], BF16)
            wd_all = A.alloc("wd_all", [128, NJ, D], BF16)
            gu2 = [A.alloc("mgu", [128, 2, 8, 256], BF16) for _ in range(2)]
            hg3 = [A.alloc("hg", [128, D], BF16) for _ in range(3)]
            yst2 = [A.alloc("yst", [128, D], F32) for _ in range(2)]
            sg2 = [A.alloc("msg", [128, 384], BF16) for _ in range(2)]
            ent2 = [A.alloc("ent", [128, MKS * 2], I32) for _ in range(2)]
            items = [(sl_, jj) for sl_ in range(NSLOT) for jj in range(NJ // 2)]
            pfs = [0]

            def prefetch(upto):
                while pfs[0] < min(upto, len(items)):
                    sl_, jj = items[pfs[0]]
                    b = pfs[0] % 2
                    col = sl_ * 14 + jj
                    em("pool", lambda e, b=b, col=col: e.indirect_dma_start(out=gu2[b][:, 0].rearrange("p c n -> p (c n)"), out_offset=None, in_=mwg_in[:, :],
                                                                          in_offset=bass.IndirectOffsetOnAxis(ap=idxGU[:, col:col + 1], axis=0)),
                       reads=["idxGU"], writes=["mgu%d" % b], dma=True)
                    em("pool", lambda e, b=b, col=col: e.indirect_dma_start(out=gu2[b][:, 1].rearrange("p c n -> p (c n)"), out_offset=None, in_=mwu_in[:, :],
                                                                          in_offset=bass.IndirectOffsetOnAxis(ap=idxGU[:, col:col + 1], axis=0)),
                       reads=["idxGU"], writes=["mgu%d" % b], dma=True)
                    pfs[0] += 1

            def load_ent(sl_):
                b = sl_ % 2
                em("sp", lambda e: e.dma_start(out=ent2[b][:].rearrange("p (k w) -> p k w", w=2), in_=L2[sl_ * MC:(sl_ + 1) * MC, :].rearrange("(k p) w -> p k w", p=128)),
                   reads=["L2"] + ["L2s%d" % z for z in range(64)], writes=["ent%d" % b], dma=True)

            def gather_k(sl_, k):
                b = sl_ % 2
                hb = k % 3
                em("pool", lambda e: e.indirect_dma_start(out=hg3[hb][:, :], out_offset=None, in_=h2d[:, :],
                                                          in_offset=bass.IndirectOffsetOnAxis(ap=ent2[b][:, 2 * k:2 * k + 1], axis=0),
                                                          ), reads=["ent%d" % b, "h2d"] + ["h2d%d" % z for z in range(32)], writes=["hg%d" % hb], dma=True)

            def trans_k(sl_, k):
                b = sl_ % 2
                hb = k % 3
                for c in range(8):
                    em("pe", lambda e, c=c: e.transpose(out=ps6[:, c * 128:(c + 1) * 128], in_=hg3[hb][:, c * 128:(c + 1) * 128], identity=ident_b[:]),
                       reads=["hg%d" % hb, "ident_b"], writes=["ps6"])
                em("act", lambda e: e.activation(out=hTg[b][:, :, k * 128:(k + 1) * 128], in_=ps6[:].rearrange("p (c q) -> p c q", q=128), func=AF.Copy),
                   reads=["ps6"], writes=["hTg%d" % b])

            load_ent(0)
            for k in range(MKS):
                gather_k(0, k)
                trans_k(0, k)
            gi_ = [0]
            oi_ = [0]
            if SKIP:
                P.pred_ap = Ui[0:1, 0:1]
                P.pred_regs = {"pe": st.enter_context(nc.tensor.register("uU_pe")), "act": st.enter_context(nc.scalar.register("uU_act")),
                               "dve": st.enter_context(nc.vector.register("uU_dve")), "pool": st.enter_context(nc.gpsimd.register("uU_pool")),
                               "sp": st.enter_context(nc.sync.register("uU_sp"))}
            for sl_ in range(NSLOT):
                if SKIP and sl_ >= MINU:
                    P.site(sl_, reads=["Ui"])
                b = sl_ % 2
                hT_ = hTg[b]
                hk = "hTg%d" % b
                if sl_ + 1 < NSLOT:
                    load_ent(sl_ + 1)
                for q in range(4):
                    col = sl_ * 4 + q
                    em("pool", lambda e, q=q, col=col: e.indirect_dma_start(out=wd_all[:, q * 7:(q + 1) * 7, :].rearrange("p j n -> p (j n)"), out_offset=None, in_=mwd_in[:, :],
                                                                          in_offset=bass.IndirectOffsetOnAxis(ap=idxD[:, col:col + 1], axis=0)),
                       reads=["idxD"], writes=["wd_all"], dma=True)
                for jj in range(NJ // 2):
                    prefetch(sl_ * (NJ // 2) + jj + 2)
                    gb_ = (sl_ * (NJ // 2) + jj) % 2
                    if sl_ + 1 < NSLOT:
                        if 1 <= jj <= MKS:
                            gather_k(sl_ + 1, jj - 1)
                        if 2 <= jj <= MKS + 1:
                            trans_k(sl_ + 1, jj - 2)
                    for jh in range(2):
                        j = jj * 2 + jh
                        for tt in range(MC // 384):
                            gslot = gi_[0] % 2
                            gi_[0] += 1
                            gps, gpo, gpk = PSH[gslot]
                            ups, upo, upk = PSH[2 + gslot]
                            for c in range(8):
                                em("pe", lambda e, c=c, jh=jh, tt=tt, gps=gps, gpo=gpo, gb_=gb_, hT_=hT_: e.matmul(
                                    gps[:, gpo:gpo + 384], lhsT=gu2[gb_][:, 0, c, jh * 128:(jh + 1) * 128], rhs=hT_[:, c, tt * 384:(tt + 1) * 384],
                                    start=(c == 0), stop=(c == 7)), reads=["mgu%d" % gb_, hk], writes=[gpk])
                            for c in range(8):
                                em("pe", lambda e, c=c, jh=jh, tt=tt, ups=ups, upo=upo, gb_=gb_, hT_=hT_: e.matmul(
                                    ups[:, upo:upo + 384], lhsT=gu2[gb_][:, 1, c, jh * 128:(jh + 1) * 128], rhs=hT_[:, c, tt * 384:(tt + 1) * 384],
                                    start=(c == 0), stop=(c == 7)), reads=["mgu%d" % gb_, hk], writes=[upk])
                            sg = sg2[gslot]
                            em("act", lambda e, sg=sg, gps=gps, gpo=gpo: e.activation(out=sg[:], in_=gps[:, gpo:gpo + 384], func=AF.Silu),
                               reads=[gpk], writes=["msg%d" % gslot])
                            em("dve", lambda e, sg=sg, ups=ups, upo=upo, j=j, tt=tt: e.tensor_tensor(
                                out=inter[:, j, tt * 384:(tt + 1) * 384], in0=ups[:, upo:upo + 384], in1=sg[:], op=ALU.mult),
                                reads=[upk, "msg%d" % gslot], writes=["minter"])
                for k in range(MKS):
                    ys = oi_[0] % 2
                    oi_[0] += 1
                    yst = yst2[ys]
                    gcol = ent2[b][:, 2 * k + 1:2 * k + 2].bitcast(F32)
                    for half in range(2):
                        ops_, opk = (ps4, "ps4") if half == 0 else (ps5, "ps5")
                        for j in range(NJ):
                            em("pe", lambda e, j=j, k=k, half=half, ops_=ops_: e.matmul(
                                ops_[:, 0:512], lhsT=inter[:, j, k * 128:(k + 1) * 128], rhs=wd_all[:, j, half * 512:(half + 1) * 512],
                                start=(j == 0), stop=(j == NJ - 1)), reads=["minter", "wd_all"], writes=[opk])
                        em("dve", lambda e, yst=yst, ops_=ops_, half=half, gcol=gcol: e.tensor_scalar(
                            out=yst[:, half * 512:(half + 1) * 512], in0=ops_[:, 0:512], scalar1=gcol, scalar2=None, op0=ALU.mult),
                            reads=[opk, "ent%d" % b], writes=["yst%d" % ys])
                    em("pool", lambda e, yst=yst, k=k, b=b: e.indirect_dma_start(
                        out=macc[:, :], out_offset=bass.IndirectOffsetOnAxis(ap=ent2[b][:, 2 * k:2 * k + 1], axis=0), in_=yst[:, :], in_offset=None,
                        compute_op=ALU.add), reads=["yst%d" % ys, "ent%d" % b] + ["maccz%d" % z for z in range(33)], writes=["macc"], dma=True)
            if SKIP:
                P.end_sites()
            P.barrier()
            A.reset(mk0)

        def run_layer(l):
            last = l == DEPTH - 1
            xsrc3 = xT3 if l == 0 else xs3
            layer_mark = A.mark()
            kna = A.alloc("kna", [128, 3, NTOK], BF16)
            ksw = A.alloc("ksw", [128, NTOK], BF16)
            vna = A.alloc("vna", [128, 34, 6, 65], BF16)
            vsw = A.alloc("vsw", [128, 34, 2, 65], BF16)
            em("pool", lambda e, vna=vna: e.memset(vna[:, :, :, 64:65], 1.0), writes=["vna"])
            em("pool", lambda e, vsw=vsw: e.memset(vsw[:, :, :, 64:65], 1.0), writes=["vsw"])
            if stop_phase < 1:
                return
            p1_mark = A.mark()
            win = A.alloc("win", [128, 8, D_IN_EXT], BF16)
            wsrc = win_in[l].rearrange("(c p) n -> p c n", p=128)
            for c in range(8):
                em("pool", lambda e, c=c, wsrc=wsrc, win=win: e.dma_start(out=win[:, c, :], in_=wsrc[:, c, :]), writes=["win"], dma=True)
            xt2 = [A.alloc("xt", [128, 8, 512], F32)] * 2
            sq = A.alloc("sq", [128, 8, 512], BF16)
            hT2 = [A.alloc("hT", [128, 8, 512], BF16) for _ in range(2)]
            rstd = A.alloc("rstd", [128, 512], F32)
            tmp2 = [A.alloc("tmp", [128, 512], F32) for _ in range(2)]
            qst2 = [A.alloc("qst", [128, 10, 512], BF16)] * 2
            ah = A.alloc("ah", [128, 2, 512], F32)
            rC2 = [A.alloc("rC", [128, 512], F32) for _ in range(2)]
            rS2 = [A.alloc("rS", [128, 512], F32) for _ in range(2)]
            r1 = [A.alloc("r1", [128, 512], F32) for _ in range(2)]
            r2 = [A.alloc("r2", [128, 512], F32) for _ in range(2)]
            pfi = [0]

            def next_pf():
                i = pfi[0] % 3
                pfi[0] += 1
                return PSH[i]

            tiles = [(ti * 512, 512, 0) for ti in range(8)] + [(SEQ, CTX, 1)]
            def p1_norm(it):
                t0, TT, w = tiles[it]
                s = it % 2
                xt, hT = xt2[s], hT2[s]
                a, b, g = modcols(l, 0, w)
                em("sp", lambda e: e.dma_start(out=xt[:, :, 0:TT], in_=xsrc3[:, :, t0:t0 + TT]),
                   reads=["xs%d" % (t0 // 512)], writes=["xt"], dma=True)
                if w == 0:
                    em("sp", lambda e: e.dma_start(out=rC2[s][:], in_=ropeC_in[:, t0:t0 + 512]), writes=["rC%d" % s], dma=True)
                    em("sp", lambda e: e.dma_start(out=rS2[s][:], in_=ropeS_in[:, t0:t0 + 512]), writes=["rS%d" % s], dma=True)
                norm_mod(xt, "xt", TT, a, b, sq, "sq", rstd, "rstd", tmp2, ["tmp0", "tmp1"],
                         lambda c: hT[:, c, 0:TT], "hT%d" % s, ps7, "ps7")

            p1_norm(0)
            for it, (t0, TT, w) in enumerate(tiles):
                s = it % 2
                xt, hT, qst = xt2[s], hT2[s], qst2[s]
                xk, hk, qk = "xt", "hT%d" % s, "qst"
                rope = (w == 0)

                def proj(n, hT=hT, TT=TT, hk=hk):
                    pst, po, pk = next_pf()
                    for c in range(8):
                        em("pe", lambda e, c=c, pst=pst, po=po: e.matmul(pst[:, po:po + TT], lhsT=win[:, c, n * 128:(n + 1) * 128],
                                                                      rhs=hT[:, c, 0:TT], start=(c == 0), stop=(c == 7)),
                           reads=["win", hk], writes=[pk])
                    return pst[:, po:po + TT], pk

                if P1 < 1:
                    continue
                for n in (0, 1):
                    pa, pk = proj(n)
                    em("act", lambda e, pa=pa, n=n, TT=TT: e.activation(out=ah[:, n, 0:TT], in_=pa, func=AF.Copy), reads=[pk], writes=["ah"])
                for n in (2, 3):
                    pa, pk = proj(n)
                    em("act", lambda e, pa=pa, n=n, TT=TT, qst=qst: e.activation(out=qst[:, n, 0:TT], in_=pa, func=AF.Copy), reads=[pk], writes=[qk])
                for n in (4, 5):
                    pa, pk = proj(n)
                    em("dve", lambda e, pa=pa, n=n, TT=TT, qst=qst: e.tensor_tensor(out=qst[:, n - 4, 0:TT], in0=pa, in1=ah[:, n - 4, 0:TT], op=ALU.mult),
                       reads=[pk, "ah"], writes=[qk])
                if it + 1 < len(tiles):
                    p1_norm(it + 1)
                if P1 < 2:
                    continue
                for n in (6, 7, 8):
                    pa, pk = proj(n)
                    em("act", lambda e, pa=pa, n=n, TT=TT, qst=qst: e.activation(out=qst[:, n - 2, 0:TT], in_=pa, func=AF.Copy), reads=[pk], writes=[qk])
                for n in (15, 16, 17):
                    pa, pk = proj(n)
                    em("act", lambda e, pa=pa, n=n, TT=TT, t0=t0, kna=kna: e.activation(out=kna[:, n - 15, t0:t0 + TT], in_=pa, func=AF.Copy),
                       reads=[pk], writes=["kna"])

                def rope_out(n, ns, outap, okey, ri, s=s, rope=rope):
                    pa, pk = proj(n)
                    if not rope:
                        em("act", lambda e: e.activation(out=outap, in_=pa, func=AF.Copy), reads=[pk], writes=[okey])
                        return
                    pb, pkb = proj(ns)
                    i = ri % 2
                    em("dve", lambda e: e.tensor_tensor(out=r1[i][:], in0=pa, in1=rC2[s][:], op=ALU.mult), reads=[pk, "rC%d" % s], writes=["r1%d" % i])
                    em("dve", lambda e: e.tensor_tensor(out=r2[i][:], in0=pb, in1=rS2[s][:], op=ALU.mult), reads=[pkb, "rS%d" % s], writes=["r2%d" % i])
                    em("pool", lambda e: e.tensor_tensor(out=outap, in0=r1[i][:], in1=r2[i][:], op=ALU.add), reads=["r1%d" % i, "r2%d" % i], writes=[okey])

                if P1 < 3:
                    continue
                for j in range(3):
                    rope_out(9 + j, 12 + j, qst[:, 7 + j, 0:TT], qk, j)
                rope_out(18, 19, ksw[:, t0:t0 + TT], "ksw", 3)
                if P1 < 4:
                    continue
                em("sp", lambda e, qst=qst, t0=t0, TT=TT: e.dma_start(out=qd3[:, :, t0:t0 + TT], in_=qst[:, :, 0:TT]),
                   reads=[qk], writes=["qd%d" % (t0 // 512)], dma=True)
                for sub in range(TT // 128 if P1 >= 5 else 0):
                    pst, po, pk = next_pf()
                    for c in range(8):
                        em("pe", lambda e, c=c, pst=pst, po=po, sub=sub, hT=hT: e.matmul(
                            pst[:, po:po + 512], lhsT=hT[:, c, sub * 128:(sub + 1) * 128], rhs=win[:, c, 2560:3072],
                            start=(c == 0), stop=(c == 7)), reads=["win", hk], writes=[pk])
                    vch = t0 // 128 + sub
                    PV_ = int(os.environ.get("PV", "3"))
                    if PV_ >= 1:
                      em("act", lambda e, pst=pst, po=po, vch=vch, vna=vna: e.activation(
                        out=vna[:, vch, :, 0:64], in_=pst[:, po:po + 384].rearrange("p (h d) -> p h d", d=64), func=AF.Copy),
                        reads=[pk], writes=["vna"])
                    if PV_ >= 2:
                      em("act", lambda e, pst=pst, po=po, vch=vch, vsw=vsw: e.activation(
                        out=vsw[:, vch, :, 0:64], in_=pst[:, po + 384:po + 512].rearrange("p (h d) -> p h d", d=64), func=AF.Copy),
                        reads=[pk], writes=["vsw"])
            P.barrier()
            A.reset(p1_mark)
            if stop_phase < 2:
                return

            Eb = A.alloc("Eb", [128, 6, NT * 128], BF16)
            est = A.alloc("est", [128, NT * 128], F32)
            for h in range(6):
                em("sp", lambda e, h=h: e.dma_start(out=est[:], in_=rpbg_in[l, h]), writes=["est"], dma=True)
                em("act", lambda e: e.activation(out=est[:], in_=est[:], func=AF.Exp), reads=["est"], writes=["est"])
                em("dve", lambda e, h=h, Eb=Eb: e.tensor_tensor(out=Eb[:, h, :], in0=est[:], in1=maskNA[:], op=ALU.mult),
                   reads=["est", "maskNA"], writes=["Eb"])
            wout = A.alloc("wout", [128, 8, D], BF16)
            wsrc = wout_in[l].rearrange("(c p) n -> p c n", p=128)
            em("pool", lambda e, wsrc=wsrc, wout=wout: e.dma_start(out=wout[:], in_=wsrc), writes=["wout"], dma=True)
            for c in range(8):
                em("dve", lambda e, c=c, wout=wout: e.tensor_scalar(out=wout[:, c, :], in0=wout[:, c, :], scalar1=gn[:, (l * 3 + 2) * 8 + c:(l * 3 + 2) * 8 + c + 1],
                                                              scalar2=None, op0=ALU.mult), reads=["wout", "gn"], writes=["wout"])
            xt = A.alloc("xt", [128, 8, 512], F32)
            xo = A.alloc("xo", [128, 8, 512], F32)
            ut = A.alloc("ut", [128, 2, 514], BF16)
            abt = A.alloc("abt", [128, 2, 512], BF16)
            qn = A.alloc("qn", [128, 6, 512], BF16)
            cv1 = A.alloc("cv1", [128, 512], F32)
            ya = A.alloc("ya", [128, 2, 512], F32)
            sqa = A.alloc("sqa", [128, 2, 512], BF16)
            rsa = A.alloc("rsa", [128, 512], F32)
            yT = A.alloc("yT", [128, 8, 512], BF16)
            pt3 = [A.alloc("pt", [128, 896], BF16) for _ in range(3)]
            yg = [A.alloc("yg", [128, 6, 64], F32) for _ in range(2)]
            ygb = [A.alloc("ygb", [128, 384], BF16) for _ in range(2)]
            junk = A.alloc("junk", [128, 384], F32)
            st8 = A.alloc("st8", [128, 16], F32)
            pti = [0]
            spi = [0]

            def attn_A(qap, chunks, qkey):
                nchk = len(chunks)
                si = spi[0] % 2
                spi[0] += 1
                spt = psA if si == 0 else psB
                k0, k1 = ("psA0", "psA1") if si == 0 else ("psB0", "psB1")
                for ci, (kap, vap, m) in enumerate(chunks):
                    em("pe", lambda e, ci=ci, kap=kap: e.matmul(spt[:, ci * 128:(ci + 1) * 128], lhsT=kap, rhs=qap, start=True, stop=True),
                       reads=["kna", "ksw", qkey], writes=[k0 if ci < 4 else k1])
                pi = pti[0] % 3
                pti[0] += 1
                pt = pt3[pi]
                pk = "pt%d" % pi
                em("act", lambda e: e.activation(out=pt[:, 0:nchk * 128], in_=spt[:, 0:nchk * 128], func=AF.Exp, scale=0.125),
                   reads=[k0, k1], writes=[pk])
                ci = 0
                while ci < nchk:
                    m = chunks[ci][2]
                    if m is None:
                        ci += 1
                        continue
                    tbl, col0 = m
                    cj = ci + 1
                    while cj < nchk and chunks[cj][2] is not None and chunks[cj][2][0] is tbl and chunks[cj][2][1] == col0 + (cj - ci) * 128:
                        cj += 1
                    n = cj - ci
                    em("dve", lambda e, ci=ci, n=n, tbl=tbl, col0=col0: e.tensor_tensor(
                        out=pt[:, ci * 128:(ci + n) * 128], in0=pt[:, ci * 128:(ci + n) * 128], in1=tbl[col0:col0 + n * 128], op=ALU.mult),
                        reads=[pk, "Eb", "maskLR"], writes=[pk])
                    ci = cj
                return pt, pk

            def attn_B(ctx, chunks, ops_ap, opk, after):
                pt, pk = ctx
                nchk = len(chunks)
                for ci, (kap, vap, m) in enumerate(chunks):
                    em("pe", lambda e, ci=ci, vap=vap: e.matmul(ops_ap, lhsT=pt[:, ci * 128:(ci + 1) * 128], rhs=vap,
                                                             start=(ci == 0), stop=(ci == nchk - 1)),
                       reads=[pk, "vna", "vsw"], writes=[opk])
                if after is not None:
                    return after()
                return None

            class Tbl:
                def __init__(self, f):
                    self.f = f

                def __getitem__(self, sl):
                    return self.f(sl)

            EbT = [Tbl(lambda sl, h=h: Eb[:, h, sl]) for h in range(6)]
            mLR = Tbl(lambda sl: maskLR[:, sl])

            def finish_group(ops, opk, gi, sinkcols, ycols0, s):
                o3 = ops[:, 0:390].rearrange("p (h d) -> p h d", d=65)
                den = st8[:, gi * 8:gi * 8 + 6]
                if sinkcols is None:
                    em("dve", lambda e: e.tensor_copy(out=den, in_=o3[:, :, 64]), reads=[opk], writes=["st8"])
                else:
                    em("dve", lambda e: e.tensor_tensor(out=den, in0=o3[:, :, 64], in1=sinkcols, op=ALU.add), reads=[opk, "esink"], writes=["st8"])
                em("dve", lambda e: e.reciprocal(out=den, in_=den), reads=["st8"], writes=["st8"])
                y = yg[gi]
                yk = "yg%d" % gi
                em("dve", lambda e: e.tensor_tensor(out=y[:], in0=o3[:, :, 0:64], in1=den.unsqueeze(2).to_broadcast([128, 6, 64]), op=ALU.mult),
                   reads=[opk, "st8"], writes=[yk])
                y2 = y[:].rearrange("p h d -> p (h d)")
                ss = st8[:, gi * 8 + 6:gi * 8 + 7]
                em("dve", lambda e: e.memset(ss, 0.0), writes=["st8"])
                em("dve", lambda e: e.scalar_tensor_tensor(out=junk[:], in0=y2, scalar=1.0, in1=y2, op0=ALU.mult, op1=ALU.mult, accum_out=ss),
                   reads=[yk], writes=["junk", "st8"])
                em("act", lambda e: e.activation(out=ss, in_=ss, func=AF.Ln, scale=1.0 / 384, bias=EPS), reads=["st8"], writes=["st8"])
                em("act", lambda e: e.activation(out=ss, in_=ss, func=AF.Exp, scale=-0.5), reads=["st8"], writes=["st8"])
                yb = ygb[gi]
                ybk = "ygb%d" % gi
                em("dve", lambda e: e.tensor_scalar(out=yb[:], in0=y2, scalar1=ss, scalar2=None, op0=ALU.mult), reads=[yk, "st8"], writes=[ybk])

                def tail():
                    for j in range(3):
                        em("pe", lambda e, j=j: e.transpose(out=ps6[:, (gi * 3 + j) * 128:(gi * 3 + j + 1) * 128], in_=yb[:, j * 128:(j + 1) * 128], identity=ident_b[:]),
                           reads=[ybk, "ident_b"], writes=["ps6_%d" % gi])
                    em("act", lambda e: e.activation(out=yT[:, ycols0:ycols0 + 3, s * 128:(s + 1) * 128],
                                                     in_=ps6[:, gi * 384:(gi + 1) * 384].rearrange("p (j q) -> p j q", q=128), func=AF.Copy),
                       reads=["ps6_%d" % gi], writes=["yT"])
                return tail

            tiles2 = [(ti * 512, 512, 0) for ti in range(8)]
            if not last:
                tiles2.append((SEQ, CTX, 1))
            xt2 = [xt, xo]
            qn2 = [qn, A.alloc("qnb", [128, 6, 512], BF16)]
            ut2 = [ut, A.alloc("utb", [128, 2, 514], BF16)]
            abt2 = [abt, A.alloc("abtb", [128, 2, 512], BF16)]
            qdkeys = ["qd%d" % k for k in range(9)]

            def emit_loads(it):
                t0, TT, w = tiles2[it]
                sl = it % 2
                seq_lo, seq_hi = (0, SEQ) if w == 0 else (SEQ, NTOK)
                qn_, ut_, abt_ = qn2[sl], ut2[sl], abt2[sl]
                em("sp", lambda e: e.dma_start(out=qn_[:, :, 0:TT], in_=qd3[:, 4:10, t0:t0 + TT]), reads=qdkeys, writes=["qn%d" % sl], dma=True)
                lo = max(t0 - 1, seq_lo)
                hi = min(t0 + TT + 1, seq_hi)
                if lo > t0 - 1:
                    em("pool", lambda e: e.memset(ut_[:, :, 0:1], 0.0), writes=["ut%d" % sl])
                if hi < t0 + TT + 1:
                    em("pool", lambda e: e.memset(ut_[:, :, TT + 1:TT + 2], 0.0), writes=["ut%d" % sl])
                em("sp", lambda e: e.dma_start(out=ut_[:, :, lo - (t0 - 1):hi - (t0 - 1)], in_=qd3[:, 0:2, lo:hi]),
                   reads=qdkeys, writes=["ut%d" % sl], dma=True)
                em("sp", lambda e: e.dma_start(out=abt_[:, :, 0:TT], in_=qd3[:, 2:4, t0:t0 + TT]), reads=qdkeys, writes=["abt%d" % sl], dma=True)
                xt_ = xt2[sl]
                em("sp", lambda e: e.dma_start(out=xt_[:, :, 0:TT], in_=xsrc3[:, :, t0:t0 + TT]), reads=["xs%d" % (t0 // 512)], writes=["xt%d" % sl], dma=True)

            def emit_conv(it):
                t0, TT, w = tiles2[it]
                sl = it % 2
                ut_, abt_ = ut2[sl], abt2[sl]
                uk, ak = "ut%d" % sl, "abt%d" % sl
                for ch in range(2):
                    wc = lambda k, ch=ch: convw[:, l * 6 + ch * 3 + k:l * 6 + ch * 3 + k + 1]
                    em("dve", lambda e, ch=ch, wc=wc: e.tensor_scalar(out=cv1[:, 0:TT], in0=ut_[:, ch, 1:TT + 1], scalar1=wc(1), scalar2=None, op0=ALU.mult),
                       reads=[uk, "convw"], writes=["cv1"])
                    em("dve", lambda e, ch=ch, wc=wc: e.scalar_tensor_tensor(out=cv1[:, 0:TT], in0=ut_[:, ch, 0:TT], scalar=wc(0), in1=cv1[:, 0:TT],
                                                                       op0=ALU.mult, op1=ALU.add), reads=[uk, "convw", "cv1"], writes=["cv1"])
                    em("dve", lambda e, ch=ch, wc=wc: e.scalar_tensor_tensor(out=cv1[:, 0:TT], in0=ut_[:, ch, 2:TT + 2], scalar=wc(2), in1=cv1[:, 0:TT],
                                                                       op0=ALU.mult, op1=ALU.add), reads=[uk, "convw", "cv1"], writes=["cv1"])
                    em("dve", lambda e, ch=ch: e.tensor_tensor(out=ya[:, ch, 0:TT], in0=cv1[:, 0:TT], in1=abt_[:, ch, 0:TT], op=ALU.mult),
                       reads=["cv1", ak], writes=["ya"])
                em("pool", lambda e: e.tensor_tensor(out=sqa[:, :, 0:TT], in0=ya[:, :, 0:TT], in1=ya[:, :, 0:TT], op=ALU.mult), reads=["ya"], writes=["sqa"])
                for ch in range(2):
                    em("pe", lambda e, ch=ch: e.matmul(ps7[:, 0:TT], lhsT=ones256[:], rhs=sqa[:, ch, 0:TT], start=(ch == 0), stop=(ch == 1)),
                       reads=["sqa", "ones256"], writes=["ps7"])
                em("act", lambda e: e.activation(out=rsa[:, 0:TT], in_=ps7[:, 0:TT], func=AF.Ln, bias=EPS, scale=1.0), reads=["ps7"], writes=["rsa"])
                em("act", lambda e: e.activation(out=rsa[:, 0:TT], in_=rsa[:, 0:TT], func=AF.Exp, scale=-0.5), reads=["rsa"], writes=["rsa"])
                for ch in range(2):
                    em("dve", lambda e, ch=ch: e.tensor_tensor(out=yT[:, ch, 0:TT], in0=ya[:, ch, 0:TT], in1=rsa[:, 0:TT], op=ALU.mult),
                       reads=["ya", "rsa"], writes=["yT"])

            emit_loads(0)
            for it, (t0, TT, w) in enumerate(tiles2):
                a1, b1, g1 = modcols(l, 0, w)
                xkey = "xs%d" % (t0 // 512)
                qn = qn2[it % 2]
                qkey = "qn%d" % (it % 2)
                tasks = []
                for s in range(TT // 128):
                    blk = t0 // 128 + s
                    ctxk = [(SEQ + cc * 128, 32 + cc) for cc in range(2)]
                    for h in range(6):
                        j, po = h // 2, (h % 2) * 64
                        qap = qn[po:po + 64, j, s * 128:(s + 1) * 128]
                        chunks = []
                        if w == 0:
                            for (k0r, tb) in NA_TILES[blk]:
                                chunks.append((kna[po:po + 64, j, k0r * 64:k0r * 64 + 128], vna[:, k0r // 2, h, :], (EbT[h], tb * 128)))
                        for (kc0, vch) in ctxk:
                            chunks.append((kna[po:po + 64, j, kc0:kc0 + 128], vna[:, vch, h, :], None))
                        tasks.append((qap, chunks, ps4[:, h * 65:(h + 1) * 65], "ps4",
                                      (lambda s=s: finish_group(ps4, "ps4", 0, None, 2, s)) if h == 5 else None))
                    for h in range(6):
                        gq, po = h // 3, (h // 3) * 64
                        qap = qn[po:po + 64, 3 + h % 3, s * 128:(s + 1) * 128]
                        chunks = []
                        if w == 0:
                            if blk > 0:
                                chunks.append((ksw[po:po + 64, (blk - 1) * 128:blk * 128], vsw[:, blk - 1, gq, :], (mLR, 0)))
                            chunks.append((ksw[po:po + 64, blk * 128:(blk + 1) * 128], vsw[:, blk, gq, :], None))
                            if blk < 31:
                                chunks.append((ksw[po:po + 64, (blk + 1) * 128:(blk + 2) * 128], vsw[:, blk + 1, gq, :], (mLR, 128)))
                        for (kc0, vch) in ctxk:
                            chunks.append((ksw[po:po + 64, kc0:kc0 + 128], vsw[:, vch, gq, :], None))
                        tasks.append((qap, chunks, ps5[:, h * 65:(h + 1) * 65], "ps5",
                                      (lambda s=s: finish_group(ps5, "ps5", 1, esink[:, l * 6:l * 6 + 6], 5, s)) if h == 5 else None))
                LA = 2
                DEFER = 3
                ctxs = {}
                tails = []
                for i in range(len(tasks) + LA):
                    if i < len(tasks):
                        ctxs[i] = attn_A(tasks[i][0], tasks[i][1], qkey)
                    if i - LA >= 0:
                        tk = tasks[i - LA]
                        tl = attn_B(ctxs.pop(i - LA), tk[1], tk[2], tk[3], tk[4])
                        if tl is not None:
                            tails.append((i - LA + DEFER, tl))
                        while tails and tails[0][0] <= i - LA:
                            tails.pop(0)[1]()
                    if i == 8 and it + 1 < len(tiles2):
                        emit_loads(it + 1)
                    if i == 4:
                        emit_conv(it)
                if len(tasks) <= 8 and it + 1 < len(tiles2):
                    emit_loads(it + 1)
                for _, tl in tails:
                    tl()
                for m in range(8):
                    wps, wpo, wpk = PSH[m % 4]
                    for c in range(8):
                        em("pe", lambda e, m=m, c=c, TT=TT, wps=wps, wpo=wpo: e.matmul(wps[:, wpo:wpo + TT], lhsT=wout[:, c, m * 128:(m + 1) * 128], rhs=yT[:, c, 0:TT],
                                                                  start=(c == 0), stop=(c == 7)), reads=["wout", "yT"], writes=[wpk])
                    em("dve", lambda e, m=m, TT=TT, g1=g1, xt_=xt2[it % 2], wps=wps, wpo=wpo: e.scalar_tensor_tensor(out=xt_[:, m, 0:TT], in0=wps[:, wpo:wpo + TT], scalar=g1(m), in1=xt_[:, m, 0:TT],
                                                                           op0=ALU.mult, op1=ALU.add), reads=[wpk, "xt%d" % (it % 2), "adav"], writes=["xt%d" % (it % 2)])
                em("sp", lambda e, t0=t0, TT=TT, xt_=xt2[it % 2]: e.dma_start(out=xs3[:, :, t0:t0 + TT], in_=xt_[:, :, 0:TT]), reads=["xt%d" % (it % 2)], writes=[xkey], dma=True)
            P.barrier()
            A.reset(layer_mark)
            if stop_phase < 3:
                return

            moe = (l % 2 == 1)
            if moe and SPARSE:
                moe_sparse(l)
                return
            NJ = (D_FFE if moe else D_FF) // 128
            nexp = NE if moe else 1
            TS = 2048
            hT = A.alloc("hT3", [128, 8, TS], BF16)
            m_inter = A.mark()
            inter = A.alloc("inter", [128, NJ, TS], BF16)
            combbc = A.alloc("combbc", [128, TS], F32)
            comb = A.alloc("comb", [128, 16, 8], F32)
            gu2 = [A.alloc("gu", [128, 2, 8, 256], BF16) for _ in range(2)]
            wd2 = [A.alloc("wd", [128, NJ, 128], BF16) for _ in range(2)]
            stg3 = [A.alloc("stg", [128, 512], F32) for _ in range(3)]
            sg2 = [A.alloc("sg", [128, 512], BF16) for _ in range(2)]
            diag = A.alloc("diag", [128, 128], F32)
            rt = A.alloc("rt", [128, 64], F32)
            p3_mark = A.mark()
            A.reset(m_inter)
            x3 = A.alloc("xt3", [128, 8, 512], F32)
            h32 = A.alloc("h32", [128, 8, 512], F32)
            sq3v = A.alloc("sq3", [128, 8, 512], BF16)
            rstd3v = A.alloc("rstd3", [128, 512], F32)
            tmp3v = [A.alloc("tmp3", [128, 512], F32) for _ in range(2)]
            A.reset(p3_mark)

            if moe:
                wg_e = lambda ex: mwg_in[ex]
                wu_e = lambda ex: mwu_in[ex]
                wd_e = lambda ex: mwd_in[ex]
            else:
                wg_e = lambda ex: fwg_in
                wu_e = lambda ex: fwu_in
                wd_e = lambda ex: fwd_in

            supers = [(0, TS, 0), (TS, TS, 0)]
            if not last:
                supers.append((SEQ, CTX, 1))
            items = []
            for si, (s0, T, w) in enumerate(supers):
                for ex in range(nexp):
                    for jj in range(NJ // 2):
                        items.append(("gu", si, ex, jj))
                    for m in range(8):
                        items.append(("wd", si, ex, m))
            cnt = {"gu": 0, "wd": 0}
            slot_of = {}
            for itx in items:
                slot_of[itx] = cnt[itx[0]] % 2
                cnt[itx[0]] += 1
            pf_state = [0]

            def prefetch(upto):
                while pf_state[0] < min(upto, len(items)):
                    kind, si, ex, idx = items[pf_state[0]]
                    sl = slot_of[items[pf_state[0]]]
                    if kind == "gu":
                        srcg = wg_e(ex).rearrange("(c p) n -> p c n", p=128)[:, :, idx * 256:(idx + 1) * 256]
                        srcu = wu_e(ex).rearrange("(c p) n -> p c n", p=128)[:, :, idx * 256:(idx + 1) * 256]
                        em("pool", lambda e, sl=sl, srcg=srcg: e.dma_start(out=gu2[sl][:, 0, :, :], in_=srcg), writes=["gu%d" % sl], dma=True)
                        em("pool", lambda e, sl=sl, srcu=srcu: e.dma_start(out=gu2[sl][:, 1, :, :], in_=srcu), writes=["gu%d" % sl], dma=True)
                    else:
                        src = wd_e(ex).rearrange("(j p) n -> p j n", p=128)[:, :, idx * 128:(idx + 1) * 128]
                        em("pool", lambda e, sl=sl, src=src: e.dma_start(out=wd2[sl][:], in_=src), writes=["wd%d" % sl], dma=True)
                    pf_state[0] += 1

            item_pos = {itx: i for i, itx in enumerate(items)}
            gi_ = [0]
            oi_ = [0]
            for si, (s0, T, w) in enumerate(supers):
                a2, b2, g2 = modcols(l, 1, w)
                ntile = max(T // 512, 1)
                TT = min(T, 512)
                P.barrier()
                for ti in range(ntile):
                    t0 = s0 + ti * TT
                    xkey = "xs%d" % (t0 // 512)
                    em("sp", lambda e, t0=t0, TT=TT: e.dma_start(out=x3[:, :, 0:TT], in_=xs3[:, :, t0:t0 + TT]), reads=[xkey], writes=["xt3"], dma=True)
                    if moe:
                        norm_mod(x3, "xt3", TT, a2, b2, sq3v, "sq3", rstd3v, "rstd3", tmp3v, ["tmp30", "tmp31"],
                                 lambda c, TT=TT: h32[:, c, 0:TT], "h32", ps7, "ps7")
                        em("pool", lambda e, ti=ti, TT=TT: e.tensor_copy(out=hT[:, :, ti * TT:(ti + 1) * TT], in_=h32[:, :, 0:TT]), reads=["h32"], writes=["hT3"])
                        for sub in range(TT // 128):
                            gsub = ti * 4 + sub
                            for c in range(8):
                                em("pe", lambda e, c=c, sub=sub: e.matmul(ps7[:, 0:8], lhsT=h32[:, c, sub * 128:(sub + 1) * 128], rhs=wr_sb[:, c * 8:(c + 1) * 8],
                                                                        start=(c == 0), stop=(c == 7)), reads=["h32", "wr_sb"], writes=["ps7"])
                            lg = rt[:, 0:8]
                            m8 = rt[:, 8:16]
                            mk1 = rt[:, 16:24]
                            mk2 = rt[:, 24:32]
                            dd = rt[:, 32:33]
                            ga = rt[:, 33:34]
                            gb = rt[:, 34:35]
                            em("dve", lambda e, lg=lg: e.tensor_copy(out=lg, in_=ps7[:, 0:8]), reads=["ps7"], writes=["rt"])
                            em("dve", lambda e, lg=lg, m8=m8: e.max(out=m8, in_=lg), reads=["rt"], writes=["rt"])
                            em("dve", lambda e, lg=lg, m8=m8, mk1=mk1: e.tensor_scalar(out=mk1, in0=lg, scalar1=m8[:, 0:1], scalar2=None, op0=ALU.is_equal), reads=["rt"], writes=["rt"])
                            em("dve", lambda e, lg=lg, m8=m8, mk2=mk2: e.tensor_scalar(out=mk2, in0=lg, scalar1=m8[:, 1:2], scalar2=None, op0=ALU.is_equal), reads=["rt"], writes=["rt"])
                            em("dve", lambda e, m8=m8, dd=dd: e.tensor_tensor(out=dd, in0=m8[:, 1:2], in1=m8[:, 0:1], op=ALU.subtract), reads=["rt"], writes=["rt"])
                            em("act", lambda e, dd=dd: e.activation(out=dd, in_=dd, func=AF.Exp), reads=["rt"], writes=["rt"])
                            em("dve", lambda e, dd=dd, ga=ga: e.tensor_scalar(out=ga, in0=dd, scalar1=1.0, scalar2=None, op0=ALU.add), reads=["rt"], writes=["rt"])
                            em("dve", lambda e, ga=ga: e.reciprocal(out=ga, in_=ga), reads=["rt"], writes=["rt"])
                            em("dve", lambda e, dd=dd, ga=ga, gb=gb: e.tensor_tensor(out=gb, in0=dd, in1=ga, op=ALU.mult), reads=["rt"], writes=["rt"])
                            em("dve", lambda e, mk1=mk1, ga=ga: e.tensor_scalar(out=mk1, in0=mk1, scalar1=ga, scalar2=None, op0=ALU.mult), reads=["rt"], writes=["rt"])
                            em("dve", lambda e, mk1=mk1, mk2=mk2, gb=gb, gsub=gsub: e.scalar_tensor_tensor(out=comb[:, gsub, :], in0=mk2, scalar=gb, in1=mk1, op0=ALU.mult, op1=ALU.add),
                               reads=["rt"], writes=["comb"])
                    else:
                        norm_mod(x3, "xt3", TT, a2, b2, sq3v, "sq3", rstd3v, "rstd3", tmp3v, ["tmp30", "tmp31"],
                                 lambda c, ti=ti, TT=TT: hT[:, c, ti * TT:(ti + 1) * TT], "hT3", ps7, "ps7")
                P.barrier()
                for ex in range(nexp):
                    if moe:
                        for sub in range(T // 128):
                            em("dve", lambda e, sub=sub, ex=ex: e.tensor_scalar(out=diag[:], in0=ident_f[:], scalar1=comb[:, sub, ex:ex + 1], scalar2=None, op0=ALU.mult),
                               reads=["ident_f", "comb"], writes=["diag"])
                            em("pe", lambda e, sub=sub: e.matmul(ps7[:, (sub % 4) * 128:(sub % 4 + 1) * 128], lhsT=ones_f[:], rhs=diag[:], start=True, stop=True),
                               reads=["diag", "ones_f"], writes=["ps7"])
                            if sub % 4 == 3:
                                em("act", lambda e, sub=sub: e.activation(out=combbc[:, (sub - 3) * 128:(sub + 1) * 128], in_=ps7[:, 0:512], func=AF.Copy),
                                   reads=["ps7"], writes=["combbc"])
                    for jj in range(NJ // 2):
                        itx = ("gu", si, ex, jj)
                        prefetch(item_pos[itx] + 2)
                        sl = slot_of[itx]
                        for jh in range(2):
                            j = jj * 2 + jh
                            for ti in range(ntile):
                                gslot = gi_[0] % 2
                                gi_[0] += 1
                                gps, gpo, gpk = PSH[gslot]
                                ups, upo, upk = PSH[2 + gslot]
                                for c in range(8):
                                    em("pe", lambda e, c=c, sl=sl, jh=jh, ti=ti, gps=gps, gpo=gpo, TT=TT: e.matmul(
                                        gps[:, gpo:gpo + TT], lhsT=gu2[sl][:, 0, c, jh * 128:(jh + 1) * 128], rhs=hT[:, c, ti * TT:(ti + 1) * TT],
                                        start=(c == 0), stop=(c == 7)), reads=["gu%d" % sl, "hT3"], writes=[gpk])
                                for c in range(8):
                                    em("pe", lambda e, c=c, sl=sl, jh=jh, ti=ti, ups=ups, upo=upo, TT=TT: e.matmul(
                                        ups[:, upo:upo + TT], lhsT=gu2[sl][:, 1, c, jh * 128:(jh + 1) * 128], rhs=hT[:, c, ti * TT:(ti + 1) * TT],
                                        start=(c == 0), stop=(c == 7)), reads=["gu%d" % sl, "hT3"], writes=[upk])
                                sg = sg2[gslot]
                                em("act", lambda e, sg=sg, gps=gps, gpo=gpo, TT=TT: e.activation(out=sg[:, 0:TT], in_=gps[:, gpo:gpo + TT], func=AF.Silu),
                                   reads=[gpk], writes=["sg%d" % gslot])
                                em("dve", lambda e, sg=sg, ups=ups, upo=upo, j=j, ti=ti, TT=TT: e.tensor_tensor(
                                    out=inter[:, j, ti * TT:(ti + 1) * TT], in0=ups[:, upo:upo + TT], in1=sg[:, 0:TT], op=ALU.mult),
                                    reads=[upk, "sg%d" % gslot], writes=["inter"])
                    for m in range(8):
                        itx = ("wd", si, ex, m)
                        prefetch(item_pos[itx] + 2)
                        sl = slot_of[itx]
                        for ti in range(ntile):
                            t0 = s0 + ti * TT
                            xkey = "xs%d" % (t0 // 512)
                            oslot = oi_[0] % 2
                            so = oi_[0] % 3
                            oi_[0] += 1
                            ops_, opk = (ps4, "ps4") if oslot == 0 else (ps5, "ps5")
                            for j in range(NJ):
                                em("pe", lambda e, j=j, sl=sl, ti=ti, ops_=ops_, TT=TT: e.matmul(
                                    ops_[:, 0:TT], lhsT=wd2[sl][:, j, :], rhs=inter[:, j, ti * TT:(ti + 1) * TT], start=(j == 0), stop=(j == NJ - 1)),
                                    reads=["wd%d" % sl, "inter"], writes=[opk])
                            stg = stg3[so]
                            if moe:
                                em("dve", lambda e, stg=stg, ops_=ops_, m=m, ti=ti, TT=TT, g2=g2: e.scalar_tensor_tensor(
                                    out=stg[:, 0:TT], in0=ops_[:, 0:TT], scalar=g2(m), in1=combbc[:, ti * TT:(ti + 1) * TT], op0=ALU.mult, op1=ALU.mult),
                                    reads=[opk, "combbc", "adav"], writes=["stg%d" % so])
                            else:
                                em("dve", lambda e, stg=stg, ops_=ops_, m=m, TT=TT, g2=g2: e.tensor_scalar(
                                    out=stg[:, 0:TT], in0=ops_[:, 0:TT], scalar1=g2(m), scalar2=None, op0=ALU.mult),
                                    reads=[opk, "adav"], writes=["stg%d" % so])
                            em("pool", lambda e, stg=stg, m=m, t0=t0, TT=TT: e.dma_start(out=xs3[:, m, t0:t0 + TT], in_=stg[:, 0:TT], accum_op=ALU.add),
                               reads=["stg%d" % so], writes=[xkey], dma=True)
            P.barrier()
            A.reset(layer_mark)

        for l_ in range(nlayers):
            run_layer(l_)

        if stop_phase >= 4:
            xt2 = [A.alloc("xtf", [128, 8, 512], F32) for _ in range(2)]
            sq = A.alloc("sqf", [128, 8, 512], BF16)
            rstd = A.alloc("rstdf", [128, 512], F32)
            yo2 = [A.alloc("yof", [128, 8, 512], F32) for _ in range(2)]
            mt2 = [A.alloc("mtf", [128, D], F32) for _ in range(2)]
            gf0 = DEPTH * 3 * 8
            def f_merge(ti):
                s = ti % 2
                xt, yo = xt2[s], yo2[s]
                t0 = ti * 512
                em("sp", lambda e, xt=xt, t0=t0: e.dma_start(out=xt[:], in_=xs3[:, :, t0:t0 + 512]), reads=["xs%d" % ti], writes=["xtf%d" % s], dma=True)
                if SPARSE and nlayers == DEPTH:
                    _, _, g2f = modcols(DEPTH - 1, 1, 0)
                    for sub in range(4):
                        mi = (ti * 4 + sub) % 2
                        mt = mt2[mi]
                        pX, pk0, pk1 = (psA, "psA0", "psA1") if mi == 0 else (psB, "psB0", "psB1")
                        em("sp", lambda e, mt=mt, t0=t0, sub=sub: e.dma_start(out=mt[:], in_=macc[t0 + sub * 128:t0 + (sub + 1) * 128, :]),
                           reads=["macc"], writes=["mt%d" % mi], dma=True)
                        for c in range(8):
                            em("pe", lambda e, c=c, mt=mt, pX=pX: e.transpose(out=pX[:, c * 128:(c + 1) * 128], in_=mt[:, c * 128:(c + 1) * 128], identity=ident_f[:]),
                               reads=["mt%d" % mi, "ident_f"], writes=[pk0 if c < 4 else pk1])
                        for c in range(8):
                            em("dve", lambda e, c=c, xt=xt, pX=pX, sub=sub, g2f=g2f: e.scalar_tensor_tensor(
                                out=xt[:, c, sub * 128:(sub + 1) * 128], in0=pX[:, c * 128:(c + 1) * 128], scalar=g2f(c), in1=xt[:, c, sub * 128:(sub + 1) * 128],
                                op0=ALU.mult, op1=ALU.add), reads=[pk0 if c < 4 else pk1, "xtf%d" % s, "adav"], writes=["xtf%d" % s])

            def f_norm(ti):
                s = ti % 2
                xt, yo = xt2[s], yo2[s]
                t0 = ti * 512
                em("act", lambda e, xt=xt: e.activation(out=sq[:], in_=xt[:], func=AF.Square), reads=["xtf%d" % s], writes=["sqf"])
                for c in range(8):
                    em("pe", lambda e, c=c: e.matmul(ps7[:], lhsT=onesD[:], rhs=sq[:, c, :], start=(c == 0), stop=(c == 7)), reads=["sqf", "onesD"], writes=["ps7"])
                em("act", lambda e: e.activation(out=rstd[:], in_=ps7[:], func=AF.Sqrt, bias=EPS, scale=1.0), reads=["ps7"], writes=["rstdf"])
                em("dve", lambda e: e.reciprocal(out=rstd[:], in_=rstd[:]), reads=["rstdf"], writes=["rstdf"])
                for c in range(8):
                    em("dve", lambda e, c=c, xt=xt, yo=yo: e.scalar_tensor_tensor(out=yo[:, c, :], in0=xt[:, c, :], scalar=gn[:, gf0 + c:gf0 + c + 1], in1=rstd[:],
                                                                           op0=ALU.mult, op1=ALU.mult), reads=["xtf%d" % s, "rstdf", "gn"], writes=["yof%d" % s])
                em("sp", lambda e, yo=yo, t0=t0: e.dma_start(out=yT3[:, :, t0:t0 + 512], in_=yo[:]), reads=["yof%d" % s], writes=["yout%d" % ti], dma=True)

            f_merge(0)
            for ti in range(8):
                if ti + 1 < 8:
                    f_merge(ti + 1)
                f_norm(ti)
        P.finish()
        P.replay(st)
    return nc


def _pcols(v):
    v = np.asarray(v, np.float32)
    lead = v.shape[:-1]
    n = v.shape[-1] // 128
    v = v.reshape(lead + (n, 128))
    return np.ascontiguousarray(np.moveaxis(v, -1, 0))


def prep_shared(inputs):
    f = lambda k: np.asarray(inputs[k], np.float32)
    sh = {}
    sh["w_ada"] = np.ascontiguousarray(f("w_ada"))
    sh["b_ada"] = np.ascontiguousarray(_pcols(f("b_ada")).reshape(128, DEPTH * 48))
    gn = np.stack([f("g_norm1"), f("g_norm2"), f("g_mix")], axis=1)
    gn = _pcols(gn).reshape(128, DEPTH * 3 * 8)
    sh["gn"] = np.ascontiguousarray(np.concatenate([gn, _pcols(f("g_final"))], axis=1))
    sh["w_in"] = np.ascontiguousarray(f("w_in")[:, :, w_in_ext_cols()])
    cw = f("conv_w")
    cw = cw.reshape(DEPTH, 3, 2, 128).transpose(3, 0, 2, 1)
    sh["convw"] = np.ascontiguousarray(cw.reshape(128, DEPTH * 6))
    ridx, cidx, mask = na_table_indices()
    rpb = f("na_rpb")
    g = rpb[:, :, ridx, cidx]
    sh["rpbg"] = np.ascontiguousarray(g.transpose(0, 1, 3, 2, 4).reshape(DEPTH, 6, 128, NT * 128))
    sh["sink"] = np.ascontiguousarray(np.broadcast_to(f("sw_sink").reshape(1, DEPTH * 6), (128, DEPTH * 6)))
    sh["w_out"] = np.ascontiguousarray(f("w_out"))
    sh["ffn_wg"] = np.ascontiguousarray(f("ffn_w_gate")[0])
    sh["ffn_wu"] = np.ascontiguousarray(f("ffn_w_up")[0])
    sh["ffn_wd"] = np.ascontiguousarray(f("ffn_w_down")[0])
    sh["w_router"] = np.ascontiguousarray(_pcols(f("w_router")[0].T).transpose(0, 2, 1).reshape(128, 64))
    if SPARSE:
        gl = lambda w: np.ascontiguousarray(w.reshape(NE, 8, 128, 14, 256).transpose(0, 3, 2, 1, 4)).reshape(NE * 14 * 128, 2048)
        sh["moe_wg"] = gl(f("moe_w_gate")[0])
        sh["moe_wu"] = gl(f("moe_w_up")[0])
        sh["moe_wd"] = np.ascontiguousarray(f("moe_w_down")[0].reshape(NE, 4, 7, 128, D).transpose(0, 1, 3, 2, 4)).reshape(NE * 4 * 128, 7 * D)
    else:
        sh["moe_wg"] = np.ascontiguousarray(f("moe_w_gate")[0])
        sh["moe_wu"] = np.ascontiguousarray(f("moe_w_up")[0])
        sh["moe_wd"] = np.ascontiguousarray(f("moe_w_down")[0])
    kk = np.arange(128)
    ident = np.eye(128, dtype=np.float32)
    maskL = (kk[:, None] >= kk[None, :]).astype(np.float32)
    maskR = (kk[:, None] <= kk[None, :]).astype(np.float32)
    ustrict = (kk[:, None] < kk[None, :]).astype(np.float32)
    tokid = (np.arange(33)[None, :] * 128 + kk[:, None]).astype(np.float32)
    iotaGU = np.zeros((128, 112), np.float32)
    iotaGU[:, :14] = np.arange(14)[None, :] * 128 + kk[:, None]
    iotaD = np.zeros((128, 28), np.float32)
    iotaD[:, :4] = np.arange(4)[None, :] * 128 + kk[:, None]
    sh["cst"] = np.ascontiguousarray(np.concatenate([ident, maskL, maskR, mask.transpose(1, 0, 2).reshape(128, NT * 128), ustrict, tokid, iotaGU, iotaD], axis=1))
    C, S = rope_tables()
    sh["ropeC"] = C
    sh["ropeS"] = S
    return sh


def prep_core(inputs, b):
    x = np.asarray(inputs["x"][b], np.float32)
    ctx = np.asarray(inputs["ctx"][b], np.float32)
    xT = np.ascontiguousarray(np.concatenate([x.T, ctx.T], axis=1))
    c = _pcols(np.asarray(inputs["c"][b], np.float32))
    cc = _pcols(np.asarray(inputs["c_ctx"], np.float32))
    cv = np.ascontiguousarray(np.stack([c, cc], axis=2).reshape(128, 16))
    return {"xT": xT, "cv": cv}


_NC_CACHE = {}


def kernel(**inputs):
    B = inputs["x"].shape[0]
    if "nc" not in _NC_CACHE:
        _NC_CACHE["nc"] = build_nc()
    nc = _NC_CACHE["nc"]
    sh = prep_shared(inputs)
    in_maps = []
    for b in range(B):
        m = dict(sh)
        m.update(prep_core(inputs, b))
        in_maps.append(m)
    res = run_bass_kernel_spmd(nc, in_maps, core_ids=list(range(B)))
    out = np.stack([np.ascontiguousarray(r["yT"].T) for r in res.results], axis=0)
    return out.astype(np.float32)
```
